# Optimizing a Trainium2 kernel written in Bass

```python
import jax, jax.numpy as jnp
from jax import lax
import numpy as np

D_MODEL = 1024
BATCH = 8
SEQ = 4096
DEPTH = 4

HEAD_DIM = 64
ROPE_DIM = HEAD_DIM // 4
ROPE_THETA = 500000.0
NORM_EPS = 1e-6
Q_BLOCK = 128
D_FF = 4 * D_MODEL

A_HEADS = 8
A_NOPE = HEAD_DIM - ROPE_DIM
A_KV_RANK = 256
IDX_HEADS = 8
IDX_DIM = 64
TOPK_MAX = 256
B_HEADS = 8
C_HEADS = 16
C_KV_GROUPS = 2
C_HPG = C_HEADS // C_KV_GROUPS
CMP_LEN = 32
CMP_STRIDE = 16
CMP_HIDDEN = 2 * HEAD_DIM
SEL_LEN = 64
SEL_N_MAX = 16
WINDOW = 512
N_BRANCH = 3
FORCE_SCORE = 1e4

AB_WIDTHS = (A_HEADS * HEAD_DIM, A_KV_RANK, ROPE_DIM, IDX_HEADS * IDX_DIM, IDX_DIM, IDX_HEADS,
             B_HEADS * HEAD_DIM, B_HEADS * HEAD_DIM, B_HEADS * HEAD_DIM, B_HEADS)
AB_IN = sum(AB_WIDTHS)
C_WIDTHS = (C_HEADS * HEAD_DIM,) + (C_KV_GROUPS * HEAD_DIM,) * 6 + (C_HEADS * N_BRANCH,)
C_IN = sum(C_WIDTHS)
MIX_WIDTH_AB = (A_HEADS + B_HEADS) * HEAD_DIM
MIX_WIDTH_C = C_HEADS * HEAD_DIM
N_EVEN = (DEPTH + 1) // 2
N_ODD = DEPTH // 2

kernel_name = "hybrid_dsa_fox_nsa_trunk"


def rms_norm(x, g):
    xf = x.astype(jnp.float32)
    y = xf * lax.rsqrt(jnp.mean(xf * xf, axis=-1, keepdims=True) + NORM_EPS)
    return (y * g.astype(jnp.float32)).astype(x.dtype)


def rope_tables(seq):
    pos = jnp.arange(seq, dtype=jnp.float32)
    inv = ROPE_THETA ** (-jnp.arange(0, ROPE_DIM, 2, dtype=jnp.float32) / ROPE_DIM)
    ang = pos[:, None] * inv[None, :]
    return jnp.cos(ang), jnp.sin(ang)


def partial_rope(x, cos, sin):
    half = ROPE_DIM // 2
    shp = (1, x.shape[1]) + (1,) * (x.ndim - 3) + (half,)
    c = cos.reshape(shp).astype(x.dtype)
    s = sin.reshape(shp).astype(x.dtype)
    x1, x2, rest = x[..., :half], x[..., half:ROPE_DIM], x[..., ROPE_DIM:]
    return jnp.concatenate([x1 * c - x2 * s, x2 * c + x1 * s, rest], axis=-1)


def masked_softmax(s, mask):
    s = jnp.where(mask, s.astype(jnp.float32), -jnp.inf)
    m = jnp.max(s, axis=-1, keepdims=True)
    m = jnp.where(jnp.isfinite(m), m, 0.0)
    e = jnp.where(mask, jnp.exp(s - m), 0.0)
    return e / jnp.maximum(jnp.sum(e, axis=-1, keepdims=True), 1e-30)


def split_cols(y, widths):
    points, acc = [], 0
    for w in widths[:-1]:
        acc += w
        points.append(acc)
    return jnp.split(y, points, axis=-1)


def to_blocks(a):
    b, s = a.shape[:2]
    return jnp.moveaxis(a.reshape((b, s // Q_BLOCK, Q_BLOCK) + a.shape[2:]), 1, 0)


def from_blocks(a):
    n, b, q = a.shape[:3]
    return jnp.moveaxis(a, 0, 1).reshape((b, n * q) + a.shape[3:])


def dsa_mixer(q, ckv, k_rope, q_idx, k_idx, w_idx, w_uk, w_uv):
    b, s = ckv.shape[:2]
    n_keep = min(TOPK_MAX, s // 4)
    scale = HEAD_DIM ** -0.5
    q_abs = jnp.einsum('bshe,rhe->bshr', q[..., ROPE_DIM:], w_uk)
    q_rot = q[..., :ROPE_DIM]
    w_idx = w_idx * (IDX_HEADS ** -0.5 * IDX_DIM ** -0.5)
    gather = jax.vmap(lambda table, ix: table[ix])
    key_pos = jnp.arange(s)

    def block(args):
        i, qa, qr, qi, wi = args
        t = i * Q_BLOCK + jnp.arange(Q_BLOCK)
        causal = key_pos[None, :] <= t[:, None]
        rel = jax.nn.relu(jnp.einsum('bqhd,bsd->bqhs', qi, k_idx))
        score = jnp.einsum('bqhs,bqh->bqs', rel, wi).astype(jnp.float32)
        score = jnp.where(causal[None], score, -jnp.inf)
        _, idx = lax.top_k(score, n_keep)
        valid = idx <= t[None, :, None]
        c_sel = gather(ckv, idx)
        kr_sel = gather(k_rope, idx)
        logits = (jnp.einsum('bqhr,bqkr->bqhk', qa, c_sel)
                  + jnp.einsum('bqhe,bqke->bqhk', qr, kr_sel)) * scale
        p = masked_softmax(logits, valid[:, :, None, :]).astype(c_sel.dtype)
        return jnp.einsum('bqhk,bqkr->bqhr', p, c_sel)

    nblk = s // Q_BLOCK
    o_lat = from_blocks(lax.map(block, (jnp.arange(nblk), to_blocks(q_abs), to_blocks(q_rot),
                                        to_blocks(q_idx), to_blocks(w_idx))))
    return jnp.einsum('bshr,rhd->bshd', o_lat, w_uv)


def fox_mixer(q, k, v, f_logit):
    b, s = q.shape[:2]
    scale = HEAD_DIM ** -0.5
    cum = lax.cumsum(jax.nn.log_sigmoid(f_logit.astype(jnp.float32)), axis=1)
    cum_k = jnp.transpose(cum, (0, 2, 1))
    key_pos = jnp.arange(s)

    def block(args):
        i, qb, cq = args
        t = i * Q_BLOCK + jnp.arange(Q_BLOCK)
        causal = key_pos[None, :] <= t[:, None]
        logits = (jnp.einsum('bqhd,bshd->bhqs', qb, k).astype(jnp.float32) * scale
                  + jnp.transpose(cq, (0, 2, 1))[..., None] - cum_k[:, :, None, :])
        p = masked_softmax(logits, causal[None, None]).astype(v.dtype)
        return jnp.einsum('bhqs,bshd->bqhd', p, v)

    nblk = s // Q_BLOCK
    return from_blocks(lax.map(block, (jnp.arange(nblk), to_blocks(q), to_blocks(cum))))


def nsa_mixer(q, k_cmp, v_cmp, k_sel, v_sel, k_win, v_win, gate_logit,
              pe_k, pe_v, wk1, wk2, wv1, wv2):
    b, s = q.shape[:2]
    g, d = C_KV_GROUPS, HEAD_DIM
    scale = d ** -0.5
    n_cmp = (s - CMP_LEN) // CMP_STRIDE + 1
    n_selb = s // SEL_LEN
    n_keep = min(SEL_N_MAX, n_selb)
    tok = np.arange(n_cmp)[:, None] * CMP_STRIDE + np.arange(CMP_LEN)[None, :]

    def compress(x, pe, w1, w2):
        xb = x[:, tok] + pe[None, None, :, None, :]
        xb = jnp.transpose(xb, (0, 1, 3, 2, 4)).reshape(b, n_cmp, g, CMP_LEN * d)
        return jax.nn.gelu(xb @ w1) @ w2

    kc = compress(k_cmp, pe_k, wk1, wk2)
    vc = compress(v_cmp, pe_v, wv1, wv2)
    cmp_end = jnp.asarray(tok[:, -1])
    sel_start = np.arange(n_selb) * SEL_LEN
    overlap = ((tok[:, 0][None, :] <= sel_start[:, None] + SEL_LEN - 1)
               & (tok[:, -1][None, :] >= sel_start[:, None]))
    overlap = jnp.asarray(overlap, jnp.float32)
    ks_blk = jnp.transpose(k_sel.reshape(b, n_selb, SEL_LEN, g, d), (0, 3, 1, 2, 4))
    vs_blk = jnp.transpose(v_sel.reshape(b, n_selb, SEL_LEN, g, d), (0, 3, 1, 2, 4))
    kw_pad = jnp.pad(k_win, ((0, 0), (WINDOW, 0), (0, 0), (0, 0)))
    vw_pad = jnp.pad(v_win, ((0, 0), (WINDOW, 0), (0, 0), (0, 0)))
    gather2 = jax.vmap(jax.vmap(lambda table, ix: table[ix]))
    qg = q.reshape(b, s, g, C_HPG, d)
    gates = jax.nn.sigmoid(gate_logit.astype(jnp.float32)).reshape(b, s, g, C_HPG, N_BRANCH)
    blk_ids = jnp.arange(n_selb)

    def block(args):
        i, qb, gb = args
        t = i * Q_BLOCK + jnp.arange(Q_BLOCK)
        s_c = jnp.einsum('bqghd,bngd->bghqn', qb, kc) * scale
        p_c = masked_softmax(s_c, cmp_end[None, :] <= t[:, None])
        o_c = jnp.einsum('bghqn,bngd->bqghd', p_c.astype(vc.dtype), vc)
        imp = jnp.einsum('bghqn,jn->bgqj', p_c, overlap)
        cur = t // SEL_LEN
        forced = ((blk_ids[None, :] == 0) | (blk_ids[None, :] == cur[:, None])
                  | (blk_ids[None, :] == cur[:, None] - 1))
        imp = jnp.where(forced, FORCE_SCORE, imp)
        imp = jnp.where(blk_ids[None, :] * SEL_LEN <= t[:, None], imp, -jnp.inf)
        _, idx = lax.top_k(imp, n_keep)
        k_g = gather2(ks_blk, idx).reshape(b, g, Q_BLOCK, n_keep * SEL_LEN, d)
        v_g = gather2(vs_blk, idx).reshape(b, g, Q_BLOCK, n_keep * SEL_LEN, d)
        tok_pos = (idx[..., None] * SEL_LEN + jnp.arange(SEL_LEN)).reshape(b, g, Q_BLOCK, n_keep * SEL_LEN)
        valid_s = tok_pos <= t[None, None, :, None]
        s_s = jnp.einsum('bqghd,bgqkd->bghqk', qb, k_g) * scale
        p_s = masked_softmax(s_s, valid_s[:, :, None]).astype(v_g.dtype)
        o_s = jnp.einsum('bghqk,bgqkd->bqghd', p_s, v_g)
        start = i * Q_BLOCK
        kw = lax.dynamic_slice_in_dim(kw_pad, start, Q_BLOCK + WINDOW, axis=1)
        vw = lax.dynamic_slice_in_dim(vw_pad, start, Q_BLOCK + WINDOW, axis=1)
        pos = start - WINDOW + jnp.arange(Q_BLOCK + WINDOW)
        valid_w = ((pos[None, :] >= 0) & (pos[None, :] <= t[:, None])
                   & (pos[None, :] > t[:, None] - WINDOW))
        s_w = jnp.einsum('bqghd,bkgd->bghqk', qb, kw) * scale
        p_w = masked_softmax(s_w, valid_w).astype(vw.dtype)
        o_w = jnp.einsum('bghqk,bkgd->bqghd', p_w, vw)
        out = gb[..., 0:1] * o_c + gb[..., 1:2] * o_s + gb[..., 2:3] * o_w
        return out.astype(qb.dtype)

    nblk = s // Q_BLOCK
    o = from_blocks(lax.map(block, (jnp.arange(nblk), to_blocks(qg), to_blocks(gates))))
    return o.reshape(b, s, C_HEADS * d)


def ab_layer(u, w_in, b_forget, kv_norm, w_uk, w_uv, kidx_norm, w_out, cos, sin):
    b, s, _ = u.shape
    aq, ackv, akr, aqi, aki, awi, bq, bk, bv, bf = split_cols(u @ w_in, AB_WIDTHS)
    aq = partial_rope(aq.reshape(b, s, A_HEADS, HEAD_DIM), cos, sin)
    ackv = rms_norm(ackv, kv_norm)
    akr = partial_rope(akr, cos, sin)
    aqi = partial_rope(aqi.reshape(b, s, IDX_HEADS, IDX_DIM), cos, sin)
    aki = partial_rope(rms_norm(aki, kidx_norm), cos, sin)
    o_a = dsa_mixer(aq, ackv, akr, aqi, aki, awi, w_uk, w_uv)
    o_b = fox_mixer(bq.reshape(b, s, B_HEADS, HEAD_DIM), bk.reshape(b, s, B_HEADS, HEAD_DIM),
                    bv.reshape(b, s, B_HEADS, HEAD_DIM), bf + b_forget)
    o = jnp.concatenate([o_a.reshape(b, s, -1), o_b.reshape(b, s, -1)], axis=-1)
    return o @ w_out


def c_layer(u, w_in, b_gate, pe_k, pe_v, wk1, wk2, wv1, wv2, w_out, cos, sin):
    b, s, _ = u.shape
    q, kc, vc, ks, vs, kw, vw, gl = split_cols(u @ w_in, C_WIDTHS)
    kv = lambda a: a.reshape(b, s, C_KV_GROUPS, HEAD_DIM)
    q = partial_rope(q.reshape(b, s, C_HEADS, HEAD_DIM), cos, sin)
    kc = partial_rope(kv(kc), cos, sin)
    ks = partial_rope(kv(ks), cos, sin)
    kw = partial_rope(kv(kw), cos, sin)
    o = nsa_mixer(q, kc, kv(vc), ks, kv(vs), kw, kv(vw), gl + b_gate, pe_k, pe_v, wk1, wk2, wv1, wv2)
    return o @ w_out


def sqrelu_mlp(u, w1, w2):
    return jnp.square(jax.nn.relu(u @ w1)) @ w2


def setup_inputs(seed: int = 0) -> dict:
    key = jax.random.key(seed)
    ks = jax.random.split(key, 22)
    nrm = lambda k, shape, fan: jax.random.normal(k, shape, jnp.float32) * (fan ** -0.5)
    gain = lambda k, shape: 1.0 + 0.02 * jax.random.normal(k, shape, jnp.float32)
    return {
        "x": jax.random.normal(ks[0], (BATCH, SEQ, D_MODEL), jnp.float32),
        "attn_norm": gain(ks[1], (DEPTH, D_MODEL)),
        "mlp_norm": gain(ks[2], (DEPTH, D_MODEL)),
        "mlp_w1": nrm(ks[3], (DEPTH, D_MODEL, D_FF), D_MODEL),
        "mlp_w2": nrm(ks[4], (DEPTH, D_FF, D_MODEL), D_FF),
        "ab_w_in": nrm(ks[5], (N_EVEN, D_MODEL, AB_IN), D_MODEL),
        "ab_b_forget": jax.random.uniform(ks[6], (N_EVEN, B_HEADS), jnp.float32, 1.0, 6.0),
        "a_kv_norm": gain(ks[7], (N_EVEN, A_KV_RANK)),
        "a_w_uk": nrm(ks[8], (N_EVEN, A_KV_RANK, A_HEADS, A_NOPE), A_KV_RANK),
        "a_w_uv": nrm(ks[9], (N_EVEN, A_KV_RANK, A_HEADS, HEAD_DIM), A_KV_RANK),
        "a_kidx_norm": gain(ks[10], (N_EVEN, IDX_DIM)),
        "ab_w_out": nrm(ks[11], (N_EVEN, MIX_WIDTH_AB, D_MODEL), MIX_WIDTH_AB),
        "c_w_in": nrm(ks[12], (N_ODD, D_MODEL, C_IN), D_MODEL),
        "c_b_gate": 0.1 * jax.random.normal(ks[13], (N_ODD, C_HEADS * N_BRANCH), jnp.float32),
        "c_pe_k": 0.02 * jax.random.normal(ks[14], (N_ODD, CMP_LEN, HEAD_DIM), jnp.float32),
        "c_pe_v": 0.02 * jax.random.normal(ks[15], (N_ODD, CMP_LEN, HEAD_DIM), jnp.float32),
        "c_cmp_k_w1": nrm(ks[16], (N_ODD, CMP_LEN * HEAD_DIM, CMP_HIDDEN), CMP_LEN * HEAD_DIM),
        "c_cmp_k_w2": nrm(ks[17], (N_ODD, CMP_HIDDEN, HEAD_DIM), CMP_HIDDEN),
        "c_cmp_v_w1": nrm(ks[18], (N_ODD, CMP_LEN * HEAD_DIM, CMP_HIDDEN), CMP_LEN * HEAD_DIM),
        "c_cmp_v_w2": nrm(ks[19], (N_ODD, CMP_HIDDEN, HEAD_DIM), CMP_HIDDEN),
        "c_w_out": nrm(ks[20], (N_ODD, MIX_WIDTH_C, D_MODEL), MIX_WIDTH_C),
        "final_norm": gain(ks[21], (D_MODEL,)),
    }


def reference(x, attn_norm, mlp_norm, mlp_w1, mlp_w2, ab_w_in, ab_b_forget, a_kv_norm, a_w_uk,
              a_w_uv, a_kidx_norm, ab_w_out, c_w_in, c_b_gate, c_pe_k, c_pe_v, c_cmp_k_w1,
              c_cmp_k_w2, c_cmp_v_w1, c_cmp_v_w2, c_w_out, final_norm):
    cos, sin = rope_tables(x.shape[1])
    h = x
    for layer in range(DEPTH):
        u = rms_norm(h, attn_norm[layer])
        if layer % 2 == 0:
            j = layer // 2
            h = h + ab_layer(u, ab_w_in[j], ab_b_forget[j], a_kv_norm[j], a_w_uk[j], a_w_uv[j],
                             a_kidx_norm[j], ab_w_out[j], cos, sin)
        else:
            j = layer // 2
            h = h + c_layer(u, c_w_in[j], c_b_gate[j], c_pe_k[j], c_pe_v[j], c_cmp_k_w1[j],
                            c_cmp_k_w2[j], c_cmp_v_w1[j], c_cmp_v_w2[j], c_w_out[j], cos, sin)
        h = h + sqrelu_mlp(rms_norm(h, mlp_norm[layer]), mlp_w1[layer], mlp_w2[layer])
    return rms_norm(h, final_norm)
```

```python
from concourse.bass_utils import run_bass_kernel_spmd
import numpy as np
import concourse.bass as bass
import concourse.mybir as mybir

F32 = mybir.dt.float32
BF16 = mybir.dt.bfloat16
ALU = mybir.AluOpType
AF = mybir.ActivationFunctionType
AX = mybir.AxisListType

ENGINES = ("pe", "act", "dve", "pool", "sp")
EPOCH = 16000
NEPOCH = {"pe": 10, "act": 8, "dve": 10, "pool": 6, "sp": 1}
NDMA_SEM = 8


class Prog:
    def __init__(self, nc, es):
        self.nc = nc
        self.ops = []
        self.esem = {e: [es.enter_context(nc.semaphore(f"s_{e}_{k}")) for k in range(NEPOCH[e])]
                     for e in ENGINES}
        self.dsem = {e: [es.enter_context(nc.semaphore(f"d_{e}_{k}")) for k in range(NDMA_SEM)]
                     for e in ("sp", "act", "pool")}
        self.bsem = es.enter_context(nc.semaphore("bar"))
        self.cnt = {e: 0 for e in ENGINES}
        self.dcnt = {e: 0 for e in ENGINES}
        self.seen = {e: {} for e in ENGINES}
        self.nphase = 0
        self.nops_total = 0

    def op(self, eng, fn, reads=(), writes=(), dma=False):
        pbr = [r for r in reads if isinstance(r, str) and r.startswith("pb")]
        if pbr:
            reads = [r for r in reads if r not in pbr]
            writes = list(writes) + [r for r in pbr if r not in writes]
        self.ops.append(dict(eng=eng, fn=fn, reads=tuple(reads), writes=tuple(writes), dma=dma))

    def dma(self, out, in_, reads=(), writes=(), eng="sp", **kw):
        self.op(eng, lambda e: e.dma_start(out=out, in_=in_, **kw), reads, writes, dma=True)

    def analyze(self):
        ops = self.ops
        last_w = {}
        readers = {}
        for i, o in enumerate(ops):
            deps = set()
            for r in o["reads"]:
                if r in last_w:
                    deps.add(last_w[r])
            for w in o["writes"]:
                if w in last_w:
                    deps.add(last_w[w])
                for j in readers.get(w, ()):
                    deps.add(j)
            deps.discard(i)
            fd = []
            for j in deps:
                oj = ops[j]
                if (not o["dma"]) and (not oj["dma"]) and o["eng"] == "pe" and oj["eng"] == "pe":
                    continue
                fd.append(j)
            o["deps"] = fd
            for j in fd:
                ops[j]["needs_inc"] = True
            for r in o["reads"]:
                readers.setdefault(r, []).append(i)
            for w in o["writes"]:
                last_w[w] = i
                readers[w] = []
        lastop = {}
        for i, o in enumerate(ops):
            if not o["dma"]:
                lastop[o["eng"]] = i
        for e, i in lastop.items():
            ops[i]["needs_inc"] = True
        for o in ops:
            e = o["eng"]
            if o["dma"]:
                n = self.dcnt[e]
                self.dcnt[e] += 1
                o["dslot"] = n % NDMA_SEM
                o["dtarget"] = 16 * (n // NDMA_SEM + 1)
            elif o.get("needs_inc"):
                n = self.cnt[e]
                self.cnt[e] += 1
                o["epoch"] = n // EPOCH
                o["ord"] = n % EPOCH + 1
                assert o["epoch"] < NEPOCH[e], f"too many incs on {e}"

    def flush(self):
        nc = self.nc
        self.analyze()
        ops = self.ops
        self.nops_total += len(ops)
        self.nphase += 1
        phase = self.nphase
        esem, dsem, bsem = self.esem, self.dsem, self.bsem

        def run_engine(e, eng):
            seen = self.seen[e]

            def wait(sem, key, val):
                if seen.get(key, 0) >= val:
                    return
                eng.wait_ge(sem, val)
                seen[key] = val

            for o in ops:
                if o["eng"] != e:
                    continue
                if o["dma"]:
                    prev = o["dtarget"] - 16
                    if prev > 0:
                        wait(dsem[e][o["dslot"]], ("d", e, o["dslot"]), prev)
                for j in o["deps"]:
                    oj = ops[j]
                    if oj["dma"]:
                        wait(dsem[oj["eng"]][oj["dslot"]], ("d", oj["eng"], oj["dslot"]), oj["dtarget"])
                    else:
                        wait(esem[oj["eng"]][oj["epoch"]], ("e", oj["eng"], oj["epoch"]), oj["ord"])
                ins = o["fn"](eng)
                if o["dma"]:
                    ins.then_inc(dsem[e][o["dslot"]], 16)
                elif o.get("needs_inc"):
                    ins.then_inc(esem[e][o["epoch"]], 1)
            n = self.cnt[e]
            if n > 0:
                ep, od = (n - 1) // EPOCH, (n - 1) % EPOCH + 1
                wait(esem[e][ep], ("e", e, ep), od)
            if e in dsem:
                nd = self.dcnt[e]
                for k in range(NDMA_SEM):
                    tot = (nd - k + NDMA_SEM - 1) // NDMA_SEM if nd > k else 0
                    if tot > 0:
                        wait(dsem[e][k], ("d", e, k), 16 * tot)
            eng.sem_inc(bsem, 1)
            eng.wait_ge(bsem, 5 * phase)

        with nc.Block() as block:
            @block.sync
            def _(eng):
                run_engine("sp", eng)

            @block.tensor
            def _(eng):
                run_engine("pe", eng)

            @block.scalar
            def _(eng):
                run_engine("act", eng)

            @block.vector
            def _(eng):
                run_engine("dve", eng)

            @block.gpsimd
            def _(eng):
                run_engine("pool", eng)
        self.ops = []


import numpy as np
from contextlib import ExitStack

D = 1024
DFF = 4096
NEG = -30000.0
STORE_ENG = "sp"


class H:
    def __init__(self, P):
        self.P = P

    def mm(self, out, lhsT, rhs, start, stop, reads, writes, skip=False):
        kw = dict(skip_group_check=True) if skip else {}
        self.P.op("pe", lambda e: e.matmul(out, lhsT=lhsT, rhs=rhs, start=start, stop=stop, **kw), reads, writes)

    def act(self, out, in_, func, reads, writes, bias=None, scale=None, accum_out=None):
        kw = {}
        if bias is not None:
            kw["bias"] = bias
        if scale is not None:
            kw["scale"] = scale
        if accum_out is not None:
            kw["accum_out"] = accum_out
        self.P.op("act", lambda e: e.activation(out=out, in_=in_, func=func, **kw), reads, writes)

    def ts(self, eng, out, in0, s1, s2, op0, op1, reads, writes, accum_out=None):
        kw = {}
        if op1 is not None:
            kw["op1"] = op1
        if accum_out is not None:
            kw["accum_out"] = accum_out
        self.P.op(eng, lambda e: e.tensor_scalar(out=out, in0=in0, scalar1=s1, scalar2=s2, op0=op0, **kw), reads, writes)

    def tt(self, eng, out, in0, in1, op, reads, writes):
        self.P.op(eng, lambda e: e.tensor_tensor(out=out, in0=in0, in1=in1, op=op), reads, writes)

    def stt(self, eng, out, in0, scalar, in1, op0, op1, reads, writes):
        self.P.op(eng, lambda e: e.scalar_tensor_tensor(out=out, in0=in0, scalar=scalar, in1=in1, op0=op0, op1=op1),
                  reads, writes)

    def copy(self, eng, out, in_, reads, writes):
        if eng == "act":
            self.P.op("act", lambda e: e.copy(out=out, in_=in_), reads, writes)
        else:
            self.P.op(eng, lambda e: e.tensor_copy(out=out, in_=in_), reads, writes)

    def memset(self, eng, ap, val, writes):
        self.P.op(eng, lambda e: e.memset(ap, val), [], writes)

    def recip(self, out, in_, reads, writes):
        self.P.op("dve", lambda e: e.reciprocal(out=out, in_=in_), reads, writes)

    def dma(self, out, in_, reads, writes, eng=None):
        if eng is None:
            eng = STORE_ENG if type(out.tensor).__name__ == "DRamTensorHandle" else "sp"
        self.P.dma(out, in_, reads=reads, writes=writes, eng=eng)


class Ctx:
    pass


def rr(lst):
    i = [0]

    def nxt():
        v = lst[i[0] % len(lst)]
        i[0] += 1
        return v
    return nxt


import numpy as np
from contextlib import ExitStack

EPS = 1e-6


class K:
    def __init__(self, S, cfg):
        self.S = S
        self.cfg = cfg
        self.nc = bass.Bass("TRN2", target_bir_lowering=False)
        self.inputs = {}
        self.NT = S // 128

    def din(self, name, shape, dt=F32):
        t = self.nc.dram_tensor(name, list(shape), dt, kind="ExternalInput").ap()
        self.inputs[name] = (tuple(shape), dt)
        return t

    def sbuf(self, es, name, shape, dt):
        self.uid = getattr(self, "uid", 0) + 1
        return es.enter_context(self.nc.sbuf_tensor(f"{name}_u{self.uid}", shape, dt))

    def dscr(self, name, shape, dt):
        if self.cfg.get("dbg") and name in self.cfg["dbg"]:
            return self.nc.dram_tensor(name, list(shape), dt, kind="ExternalOutput").ap()
        return self.nc.dram_tensor(name, list(shape), dt).ap()

    def consts(self, es):
        nc, P, Hh = self.nc, self.P, self.Hh
        sb = lambda name, shape, dt: self.sbuf(es, name, shape, dt)
        self.ident = sb("ident", [128, 128], BF16)
        self.identf = sb("identf", [128, 128], F32)
        self.epsc = sb("epsc", [128, 1], F32)
        self.pb = [es.enter_context(nc.psum_tensor(f"pb{i}", [128, 512], F32)) for i in range(8)]
        d_ident = self.din("c_ident", [128, 128])
        Hh.dma(self.identf[:], d_ident, [], ["identf"])
        Hh.copy("dve", self.ident[:], self.identf[:], ["identf"], ["ident"])
        Hh.memset("dve", self.epsc[:], EPS, ["epsc"])
        P.flush()

    def rmsnorm_T(self, hs, sub, uT, ntok_off, gain_unused, tag, pbn, scr):
        Hh = self.Hh
        ss, rstd, ub, junk = scr["ss"], scr["rstd"], scr["ub"], scr["junk"]
        hk = scr["hkey"]
        Hh.act(junk[:], hs, AF.Square, [hk, "junk" + tag], ["junk" + tag, "ss" + tag], accum_out=ss[:, 0:1])
        Hh.act(rstd[:, 0:1], ss[:, 0:1], AF.Sqrt, ["ss" + tag, "epsc"], ["rstd" + tag], bias=self.epsc[:, 0:1], scale=1.0 / D)
        Hh.recip(rstd[:, 1:2], rstd[:, 0:1], ["rstd" + tag], ["rstd2" + tag])
        Hh.act(ub[:], hs, AF.Copy, [hk, "rstd2" + tag, "ub" + tag], ["ub" + tag], scale=rstd[:, 1:2])
        for half in range(2):
            bank = pbn()
            bk = f"pb{bank}"
            for q in range(4):
                k = half * 4 + q
                Hh.mm(self.pb[bank][:, q * 128:(q + 1) * 128], ub[:, k * 128:(k + 1) * 128], self.ident[:],
                      True, True, ["ub" + tag, "ident"], [bk])
            Hh.copy("dve", uT[:, half * 4:half * 4 + 4, ntok_off:ntok_off + 128],
                    self.pb[bank][:].rearrange("p (q t) -> p q t", q=4), [bk], [scr["uTkey"]])

    def load_weight_bf16(self, es_w, name, dram, kchunks, ncols, gain=None, colsplit=2048, tagp=""):
        nc, Hh = self.nc, self.Hh
        w = self.sbuf(es_w, name, [128, kchunks, ncols], BF16)
        stg = self.stg
        i = 0
        for k in range(kchunks):
            for c0 in range(0, ncols, colsplit):
                cw = min(colsplit, ncols - c0)
                s = stg[self.stgi % 2]
                sk = f"stg{self.stgi % 2}"
                self.stgi += 1
                Hh.dma(s[:, 0:cw], dram[k * 128:(k + 1) * 128, c0:c0 + cw], [], [sk])
                eng = "pool" if (i % 2 == 0) else "dve"
                i += 1
                if gain is not None:
                    Hh.ts(eng, w[:, k, c0:c0 + cw], s[:, 0:cw], gain[:, k:k + 1], None, ALU.mult, None,
                          [sk, "gain" + tagp], [name])
                else:
                    Hh.copy(eng, w[:, k, c0:c0 + cw], s[:, 0:cw], [sk], [name])
        return w

    def phase_mlp(self, layer, h_in, h_out):
        nc, P, Hh, S = self.nc, self.P, self.Hh, self.S
        w1d = self.w_mlp1[layer]
        w2d = self.w_mlp2[layer]
        gd = self.g_mlp[layer]
        TT = 256
        with ExitStack() as es:
            sb = lambda name, shape, dt: self.sbuf(es, name, shape, dt)
            self.stg = [sb("stg0", [128, 2048], F32), sb("stg1", [128, 2048], F32)]
            self.stgi = 0
            gain = sb("gain", [128, 8], F32)
            Hh.dma(gain[:], gd, [], ["gain"])
            w1 = self.load_weight_bf16(es, "w1", w1d, 8, DFF, gain=gain)
            w2 = self.load_weight_bf16(es, "w2", w2d, 32, D)
            hs = [sb(f"hs{i}", [128, D], F32) for i in range(4)]
            uT = [sb(f"uT{i}", [128, 8, TT], BF16) for i in range(2)]
            h1T = sb("h1T", [128, 32, TT], BF16)
            rl = [sb(f"rl{i}", [128, 512], F32) for i in range(2)]
            scr = [dict(ss=sb(f"ss{i}", [128, 1], F32), rstd=sb(f"rstd{i}", [128, 2], F32),
                        ub=sb(f"ub{i}", [128, D], BF16), junk=sb(f"junk{i}", [128, D], F32)) for i in range(2)]
            pbn = rr([0, 1, 2, 3])
            pacc = [4, 5, 6, 7]
            ntt = S // TT
            def mlp_loads(tt_):
                for sub_ in range(2):
                    hi_ = (tt_ % 2) * 2 + sub_
                    r0_ = tt_ * TT + sub_ * 128
                    Hh.dma(hs[hi_][:], h_in[r0_:r0_ + 128, :], [], [f"hs{hi_}"])
            mlp_loads(0)
            for tt in range(ntt):
                u = uT[tt % 2]
                uk = f"uT{tt % 2}"
                if tt + 1 < ntt:
                    mlp_loads(tt + 1)
                for sub in range(2):
                    hi = (tt % 2) * 2 + sub
                    hk = f"hs{hi}"
                    r0 = tt * TT + sub * 128
                    sc = scr[sub]
                    sc["hkey"] = hk
                    sc["uTkey"] = uk
                    self.rmsnorm_T(hs[hi][:], sub, u, sub * 128, None, f"m{sub}", pbn, sc)
                for fp in range(16):
                    bank = pbn()
                    bk = f"pb{bank}"
                    for q in range(2):
                        f = fp * 2 + q
                        for k in range(8):
                            Hh.mm(self.pb[bank][:, q * TT:(q + 1) * TT], w1[:, k, f * 128:(f + 1) * 128], u[:, k, :],
                                  k == 0, k == 7, ["w1", uk], [bk])
                    r = rl[fp % 2]
                    rk = f"rl{fp % 2}"
                    Hh.act(r[:], self.pb[bank][:], AF.Relu, [bk, rk], [rk])
                    Hh.tt("pool", h1T[:, fp * 2:fp * 2 + 2, :], r[:].rearrange("p (q t) -> p q t", q=2),
                          r[:].rearrange("p (q t) -> p q t", q=2), ALU.mult, [rk, "h1T"], ["h1T"])
                for sub in range(2):
                    hi = (tt % 2) * 2 + sub
                    hk = f"hs{hi}"
                    for half in range(2):
                        bank = pacc[sub * 2 + half]
                        bk = f"pb{bank}"
                        for f in range(32):
                            Hh.mm(self.pb[bank][:], h1T[:, f, sub * 128:(sub + 1) * 128], w2[:, f, half * 512:(half + 1) * 512],
                                  f == 0, f == 31, ["h1T", "w2"], [bk])
                        Hh.tt("dve", hs[hi][:, half * 512:(half + 1) * 512], hs[hi][:, half * 512:(half + 1) * 512],
                              self.pb[bank][:], ALU.add, [bk, hk], [hk])
                    r0 = tt * TT + sub * 128
                    Hh.dma(h_out[r0:r0 + 128, :], hs[hi][:], [hk], [])
            P.flush()

    def phase_final(self, h_in, out):
        nc, P, Hh, S = self.nc, self.P, self.Hh, self.S
        with ExitStack() as es:
            sb = lambda name, shape, dt: self.sbuf(es, name, shape, dt)
            gb = sb("gfin", [128, D], F32)
            Hh.dma(gb[:], self.g_final, [], ["gfin"])
            hs = [sb(f"hs{i}", [128, D], F32) for i in range(2)]
            junk = [sb(f"junk{i}", [128, D], F32) for i in range(2)]
            ss = [sb(f"ss{i}", [128, 1], F32) for i in range(2)]
            rstd = [sb(f"rstd{i}", [128, 2], F32) for i in range(2)]
            Hh.dma(hs[0][:], h_in[0:128, :], [], ["hs0"])
            for t in range(self.NT):
                i = t % 2
                hk, jk, sk, rk = f"hs{i}", f"junk{i}", f"ss{i}", f"rstd{i}"
                if t + 1 < self.NT:
                    Hh.dma(hs[(t + 1) % 2][:], h_in[(t + 1) * 128:(t + 2) * 128, :], [], [f"hs{(t + 1) % 2}"])
                Hh.act(junk[i][:], hs[i][:], AF.Square, [hk, jk], [jk, sk], accum_out=ss[i][:, 0:1])
                Hh.act(rstd[i][:, 0:1], ss[i][:, 0:1], AF.Sqrt, [sk, "epsc"], [rk], bias=self.epsc[:, 0:1], scale=1.0 / D)
                Hh.recip(rstd[i][:, 1:2], rstd[i][:, 0:1], [rk], [rk + "b"])
                Hh.stt("dve", junk[i][:], hs[i][:], rstd[i][:, 1:2], gb[:], ALU.mult, ALU.mult, [hk, rk + "b", "gfin", jk], [jk])
                Hh.dma(out[t * 128:(t + 1) * 128, :], junk[i][:], [jk], [])
            P.flush()

    def build(self):
        nc, S, cfg = self.nc, self.S, self.cfg
        layers = cfg["layers"]
        NT = self.NT
        self.x = self.din("x", [S, D])
        self.out = nc.dram_tensor("out", [S, D], F32, kind="ExternalOutput").ap()
        self.w_mlp1, self.w_mlp2, self.g_mlp = {}, {}, {}
        for l in layers:
            self.w_mlp1[l] = self.din(f"mlp_w1_{l}", [D, DFF])
            self.w_mlp2[l] = self.din(f"mlp_w2_{l}", [DFF, D])
            self.g_mlp[l] = self.din(f"mlp_g_{l}", [128, 8])
        self.g_final = self.din("g_final", [128, D])
        self.hA = self.dscr("hA", [S, D], F32)
        self.hB = self.dscr("hB", [S, D], F32)
        self.ab, self.cl = {}, {}
        if cfg.get("mixer", True):
            self.ropetab = self.din("c_ropetab", [4, 128, S])
            self.c_caus = self.din("c_caus", [128, 128])
            self.c_pow2 = self.din("c_pow2", [128, 24])
            self.c_cm = self.din("c_cm", [128, 4, 512])
            self.c_negi = self.din("c_negi", [128, 128])
            for l in layers:
                j = l // 2
                if l % 2 == 0:
                    d = {}
                    d["wab"] = self.din(f"ab_w_{j}", [D, AB_NC])
                    d["g_attn"] = self.din(f"ab_g_{j}", [128, 8])
                    d["wuk"] = self.din(f"ab_wuk_{j}", [256, 512])
                    d["wuv"] = self.din(f"ab_wuv_{j}", [256, 512])
                    d["g_kv"] = self.din(f"ab_gkv_{j}", [128, 2])
                    d["g_ki"] = self.din(f"ab_gki_{j}", [64, 2])
                    d["b_forget"] = self.din(f"ab_bf_{j}", [8, 1])
                    d["w_out"] = self.din(f"ab_wout_{j}", [D, D])
                    for nm, shp, dt in (("QA", [8, 64, S], BF16), ("KA", [8, 64, S], BF16), ("VA", [S, 512], BF16),
                                        ("QI", [8, 64, S], BF16), ("KI", [64, S], BF16), ("WI", [128, NT, 8], F32),
                                        ("BQ", [8, 66, S], BF16), ("BK", [8, 66, S], BF16), ("VB", [S, 512], BF16),
                                        ("BFT", [8, S], F32), ("NCUM", [128, NT, 8], F32), ("MB", [S, S], BF16),
                                        ("OCAT", [S, D], BF16)):
                        d[nm] = self.dscr(f"{nm}_{j}", shp, dt)
                    self.ab[j] = d
                else:
                    self.decl_c(j)
        with ExitStack() as es:
            self.P = Prog(nc, es)
            self.Hh = H(self.P)
            self.consts(es)
            h = self.x
            for l in layers:
                if cfg.get("mixer", True):
                    if l % 2 == 0:
                        self.layer_ab(l, h, self.hA)
                    else:
                        self.layer_c(l, h, self.hA)
                    h = self.hA
                self.phase_mlp(l, h, self.hB)
                h = self.hB
            self.phase_final(h, self.out)
        return nc


def host_inputs(S, cfg, inp, b):
    m = {}
    m["x"] = np.ascontiguousarray(inp["x"][b, :S])
    m["c_ident"] = np.eye(128, dtype=np.float32)
    for l in cfg["layers"]:
        m[f"mlp_w1_{l}"] = np.ascontiguousarray(inp["mlp_w1"][l])
        m[f"mlp_w2_{l}"] = np.ascontiguousarray(inp["mlp_w2"][l])
        m[f"mlp_g_{l}"] = np.ascontiguousarray(inp["mlp_norm"][l].reshape(8, 128).T)
    m["g_final"] = np.ascontiguousarray(np.broadcast_to(inp["final_norm"][None, :], (128, D)))
    if cfg.get("mixer", True):
        cq, sq = rope_tables_host(S, 0.125)
        ck, sk = rope_tables_host(S, 1.0)
        m["c_ropetab"] = np.ascontiguousarray(np.stack([cq, sq, ck, sk], 0))
        pp = np.arange(128)[:, None]
        cc = np.arange(128)[None, :]
        m["c_caus"] = np.where(cc <= pp, 0.0, -1e30).astype(np.float32)
        m["c_pow2"] = np.ascontiguousarray(np.broadcast_to((2.0 ** -(np.arange(24) + 1.0))[None, :], (128, 24))).astype(np.float32)
        c5 = np.arange(512)[None, None, :]
        r4 = np.arange(4)[None, :, None]
        m["c_cm"] = np.where(128 * r4 + pp[:, :, None] <= c5, 0.0, NEG).astype(np.float32)
        m["c_negi"] = (NEG * np.eye(128)).astype(np.float32)
        for l in cfg["layers"]:
            j = l // 2
            if l % 2 == 0:
                m[f"ab_w_{j}"] = ab_weight_layout(inp["ab_w_in"][j])[0]
                m[f"ab_g_{j}"] = np.ascontiguousarray(inp["attn_norm"][l].reshape(8, 128).T)
                wuk = np.zeros((256, 8, 64), np.float32)
                wuk[:, :, 16:] = inp["a_w_uk"][j]
                m[f"ab_wuk_{j}"] = wuk.reshape(256, 512)
                m[f"ab_wuv_{j}"] = np.ascontiguousarray(inp["a_w_uv"][j].reshape(256, 512))
                m[f"ab_gkv_{j}"] = np.ascontiguousarray(inp["a_kv_norm"][j].reshape(2, 128).T)
                g = inp["a_kidx_norm"][j]
                grot = np.zeros(64, np.float32)
                grot[0:8] = g[8:16]
                grot[8:16] = g[0:8]
                m[f"ab_gki_{j}"] = np.ascontiguousarray(np.stack([g, grot], 1))
                m[f"ab_bf_{j}"] = np.ascontiguousarray(inp["ab_b_forget"][j].reshape(8, 1))
                m[f"ab_wout_{j}"] = np.ascontiguousarray(inp["ab_w_out"][j])
            else:
                host_inputs_c(m, S, inp, l)
    return m


def attention(self, es, heads, QW, out_dram, NK, unit_load=None, tagp="at"):
    nc, P, Hh, S = self.nc, self.P, self.Hh, self.S
    sb = lambda name, shape, dt: self.sbuf(es, name, shape, dt)
    NKT = (NK + 127) // 128
    C = QW // 128
    NJ = S // QW
    qs = [sb(f"qs{i}", [128, S], BF16) for i in range(2)]
    ks = [sb(f"ks{i}", [128, NKT * 128], BF16) for i in range(2)]
    vs = [sb(f"vs{i}", [128, NKT, 65], BF16) for i in range(2)]
    PT = [sb(f"pt{i}", [128, QW], BF16) for i in range(3)]
    ot = [sb(f"ot{i}", [128, C, 64], BF16) for i in range(2)]
    rc = [sb(f"rc{i}", [128, 2 * C], F32) for i in range(2)]
    for i in range(2):
        Hh.memset("pool", vs[i][:, :, 64:65], 1.0, [f"vs{i}"])

    def load_head(hi):
        hd = heads[hi]
        p = hi % 2
        Kd = hd["Kd"]
        Kb = hd.get("Kbase", Kd)
        Hh.dma(qs[p][0:Kb, :], hd["q"], [], [f"qs{p}"])
        Hh.dma(ks[p][0:Kb, 0:NK], hd["k"], [], [f"ks{p}"])
        if hd.get("q_extra") is not None:
            Hh.dma(qs[p][Kb:Kd, :], hd["q_extra"], [], [f"qs{p}"])
            Hh.dma(ks[p][Kb:Kd, 0:NK], hd["k_extra"], [], [f"ks{p}"])
        nfull = NK // 128
        if nfull > 0:
            Hh.dma(vs[p][:, 0:nfull, 0:64], hd["v"][0:nfull * 128, :].rearrange("(n p) c -> p n c", p=128), [], [f"vs{p}"])
        rem = NK - nfull * 128
        if rem > 0:
            Hh.dma(vs[p][0:rem, nfull, 0:64], hd["v"][nfull * 128:NK, :], [], [f"vs{p}"])

    work = []
    units = []
    for hi, hd in enumerate(heads):
        for j in range(NJ):
            kts = hd["kts"](j)
            if not kts:
                continue
            u = len(units)
            units.append((hi, j, kts))
            for n, kt in enumerate(kts):
                work.append((u, hi, j, kt, n == 0, n == len(kts) - 1))
    sbanks = rr([0, 1, 2, 3, 4, 5])
    wstate = {}

    def emit_qk(w):
        u, hi, j, kt, first, last = w
        hd = heads[hi]
        p = hi % 2
        Kd = hd["Kd"]
        kr = min(128, NK - kt * 128)
        bank = sbanks()
        bk = f"pb{bank}"
        terms = hd["terms"](j, kt, u % 3) if hd.get("terms") else []
        Hh.mm(self.pb[bank][0:kr, 0:QW], ks[p][0:Kd, kt * 128:kt * 128 + kr], qs[p][0:Kd, j * QW:(j + 1) * QW],
              True, len(terms) == 0, [f"ks{p}", f"qs{p}"], [bk])
        for ti, (l, r, keys) in enumerate(terms):
            Hh.mm(self.pb[bank][0:kr, 0:QW], l, r, False, ti == len(terms) - 1, list(keys), [bk])
        wstate[w] = bank

    def emit_rest(w, idx):
        u, hi, j, kt, first, last = w
        hd = heads[hi]
        p = hi % 2
        kr = min(128, NK - kt * 128)
        bank = wstate.pop(w)
        bk = f"pb{bank}"
        pi = idx % 3
        kb = hd["kbias"](kt) if hd.get("kbias") else None
        if kb is not None and kr < 128:
            kb = kb[0:kr, :]
        rd = [bk, f"pt{pi}"] + (["kbias"] if kb is not None else [])
        Hh.act(PT[pi][0:kr, 0:QW], self.pb[bank][0:kr, 0:QW], AF.Exp, rd, [f"pt{pi}"], bias=kb)
        ab = 6 + (u % 2)
        for c in range(C):
            Hh.mm(self.pb[ab][:, c * 65:(c + 1) * 65], PT[pi][0:kr, c * 128:(c + 1) * 128], vs[p][0:kr, kt, 0:65],
                  first and c == 0, last, [f"pt{pi}", f"vs{p}"], [f"pb{ab}"], skip=True)
        if last:
            oi = u % 2
            accv = self.pb[ab][:, 0:C * 65].rearrange("p (c e) -> p c e", e=65)
            Hh.ts("dve", rc[oi][:, 0:C], accv[:, :, 64], 1e-30, None, ALU.max, None, [f"pb{ab}"], [f"rc{oi}"])
            Hh.recip(rc[oi][:, C:2 * C], rc[oi][:, 0:C], [f"rc{oi}"], [f"rc{oi}"])
            for c in range(C):
                if hd.get("gate"):
                    Hh.tt("dve", rc[oi][:, C + c:C + c + 1], rc[oi][:, C + c:C + c + 1], hd["gate"](j, c), ALU.mult,
                          [f"rc{oi}", "gate"], [f"rc{oi}"])
                Hh.ts("dve", ot[oi][:, c, :], accv[:, c, 0:64], rc[oi][:, C + c:C + c + 1], None, ALU.mult, None,
                      [f"pb{ab}", f"rc{oi}", f"ot{oi}"], [f"ot{oi}"])
            oc = hd["ocol"]
            Hh.dma(out_dram[j * QW:(j + 1) * QW, oc:oc + 64].rearrange("(c p) d -> p c d", p=128), ot[oi][:], [f"ot{oi}"], [])

    LOOK = 2
    loaded = set()
    started_units = set()
    pending = []
    cnt = [0]

    def rest(w):
        emit_rest(w, cnt[0])
        cnt[0] += 1

    for w in work:
        u, hi, j, kt, first, last = w
        if hi not in loaded:
            while pending and pending[0][1] <= hi - 2:
                rest(pending.pop(0))
            load_head(hi)
            loaded.add(hi)
        if u not in started_units:
            started_units.add(u)
            if unit_load is not None:
                if u == 0:
                    unit_load(units[0][0], units[0][1], 0)
                if u + 1 < len(units):
                    unit_load(units[u + 1][0], units[u + 1][1], (u + 1) % 3)
        emit_qk(w)
        pending.append(w)
        while len(pending) > LOOK:
            rest(pending.pop(0))
        if hi + 1 < len(heads) and (hi + 1) not in loaded and all(pw[1] >= hi for pw in pending):
            load_head(hi + 1)
            loaded.add(hi + 1)
    while pending:
        rest(pending.pop(0))


K.attention = attention


AB_COLS = dict(aq=(0, 512), ackv=(512, 768), akr=(768, 784), aqi=(784, 1296), aki=(1296, 1360), awi=(1360, 1368),
               bq=(1368, 1880), bk=(1880, 2392), bv=(2392, 2904), bf=(2904, 2912))


def rot_cols(w, nheads, hd):
    w3 = w.reshape(w.shape[0], nheads, hd)
    o = np.zeros_like(w3)
    o[:, :, 0:8] = w3[:, :, 8:16]
    o[:, :, 8:16] = w3[:, :, 0:8]
    return o.reshape(w.shape)


def ab_weight_layout(w_in):
    g = lambda n: w_in[:, AB_COLS[n][0]:AB_COLS[n][1]]
    blocks = [("aq", g("aq")), ("aq_r", rot_cols(g("aq"), 8, 64)), ("aqi", g("aqi")), ("aqi_r", rot_cols(g("aqi"), 8, 64)),
              ("ackv", g("ackv")), ("akr", g("akr")), ("akr_r", rot_cols(g("akr"), 1, 16)),
              ("aki", g("aki")), ("aki_r", rot_cols(g("aki"), 1, 64)), ("awi", g("awi")),
              ("bq", g("bq")), ("bk", g("bk")), ("bv", g("bv")), ("bf", g("bf"))]
    offs, c = {}, 0
    for n, b in blocks:
        offs[n] = c
        c += b.shape[1]
    return np.ascontiguousarray(np.concatenate([b for _, b in blocks], axis=1)), offs, c


AB_OFFS = ab_weight_layout(np.zeros((1, 2912), np.float32))[1]
AB_NC = ab_weight_layout(np.zeros((1, 2912), np.float32))[2]


def rope_tables_host(S, scale):
    pos = np.arange(S, dtype=np.float32)
    inv = (500000.0 ** (-np.arange(0, 16, 2, dtype=np.float32) / 16)).astype(np.float32)
    ang = pos[None, :] * inv[:, None]
    c, s = np.cos(ang).astype(np.float32), np.sin(ang).astype(np.float32)
    C = np.ones((64, S), np.float32)
    Sn = np.zeros((64, S), np.float32)
    C[0:8] = c
    C[8:16] = c
    Sn[0:8] = -s
    Sn[8:16] = s
    C *= scale
    Sn *= scale
    return np.concatenate([C, C], 0), np.concatenate([Sn, Sn], 0)


def phase_proj_ab(self, layer, h_in):
    nc, P, Hh, S = self.nc, self.P, self.Hh, self.S
    j = layer // 2
    d = self.ab[j]
    O = AB_OFFS
    NQ = S // 512
    with ExitStack() as es:
        sb = lambda name, shape, dt: self.sbuf(es, name, shape, dt)
        self.stg = [sb("stg0", [128, 2048], F32), sb("stg1", [128, 2048], F32)]
        self.stgi = 0
        gain = sb("gain", [128, 8], F32)
        Hh.dma(gain[:], d["g_attn"], [], ["gain"])
        w = self.load_weight_bf16(es, "wab", d["wab"], 8, AB_NC, gain=gain)
        wuk = self.load_weight_bf16(es, "wuk", d["wuk"], 2, 512)
        wuv = self.load_weight_bf16(es, "wuv", d["wuv"], 2, 512)
        gkv = sb("gkv", [128, 2], F32)
        gki = sb("gki", [64, 2], F32)
        Hh.dma(gkv[:], d["g_kv"], [], ["gkv"])
        Hh.dma(gki[:], d["g_ki"], [], ["gki"])
        ones = sb("ones", [128, 128], BF16)
        Hh.memset("pool", ones[:], 1.0, ["ones"])
        hs = [sb(f"hs{i}", [128, D], F32) for i in range(4)]
        uT = [sb(f"uT{i}", [128, 8, 512], BF16) for i in range(2)]
        scr = [dict(ss=sb(f"ss{i}", [128, 1], F32), rstd=sb(f"rstd{i}", [128, 2], F32),
                    ub=sb(f"ub{i}", [128, D], BF16), junk=sb(f"junk{i}", [128, D], F32)) for i in range(2)]
        tabs = [sb(f"tab{i}", [128, 4, 512], F32) for i in range(2)]
        tmpA = [sb(f"tA{i}", [128, 512], F32) for i in range(3)]
        tmpB = [sb(f"tB{i}", [128, 512], F32) for i in range(3)]
        obf = [sb(f"ob{i}", [128, 512], BF16) for i in range(4)]
        sq = [sb(f"sq{i}", [128, 512], BF16) for i in range(2)]
        rbc = sb("rbc", [128, 512], F32)
        rbc2 = sb("rbc2", [128, 512], F32)
        ckvT = sb("ckvT", [128, 2, 512], BF16)
        akrT = sb("akrT", [16, 512], BF16)
        wis = sb("wis", [128, 4, 8], F32)
        pbn = rr([0, 1, 2, 3, 4, 5])
        cnt = dict(t=0, o=0)

        def fm(u, uk, c0, M):
            bank = pbn()
            for k in range(8):
                Hh.mm(self.pb[bank][0:M, :], w[:, k, c0:c0 + M], u[:, k, :], k == 0, k == 7, ["wab", uk], [f"pb{bank}"])
            return bank

        def nxt_ob():
            i = cnt["o"] % 4
            cnt["o"] += 1
            return obf[i], f"ob{i}"

        def rope_chunk(u, uk, cX, cR, M, tab, tk, ci, si, dest):
            bX = fm(u, uk, cX, M)
            bR = fm(u, uk, cR, M)
            i = cnt["t"] % 3
            cnt["t"] += 1
            Hh.tt("dve", tmpA[i][0:M, :], self.pb[bX][0:M, :], tab[0:M, ci, :], ALU.mult, [f"pb{bX}", tk, f"tA{i}"], [f"tA{i}"])
            Hh.tt("dve", tmpB[i][0:M, :], self.pb[bR][0:M, :], tab[0:M, si, :], ALU.mult, [f"pb{bR}", tk, f"tB{i}"], [f"tB{i}"])
            o, ok = nxt_ob()
            Hh.tt("pool", o[0:M, :], tmpA[i][0:M, :], tmpB[i][0:M, :], ALU.add, [f"tA{i}", f"tB{i}", ok], [ok])
            if dest is not None:
                for (dap, r0, r1) in dest:
                    Hh.dma(dap, o[r0:r1, :], [ok], [])
            return o, ok

        for t in range(NQ):
            u, uk = uT[t % 2], f"uT{t % 2}"
            tab, tk = tabs[t % 2], f"tab{t % 2}"
            ts_ = slice(t * 512, (t + 1) * 512)
            if t == 0:
                Hh.dma(tab[:], self.ropetab[:, :, ts_].rearrange("f p t -> p f t"), [], [tk])
                for sub in range(4):
                    Hh.dma(hs[sub][:], h_in[sub * 128:(sub + 1) * 128, :], [], [f"hs{sub}"])
            for sub in range(4):
                sc = scr[sub % 2]
                sc["hkey"] = f"hs{sub}"
                sc["uTkey"] = uk
                self.rmsnorm_T(hs[sub][:], sub, u, sub * 128, None, f"p{sub % 2}", pbn, sc)
            if t + 1 < NQ:
                tsn = slice((t + 1) * 512, (t + 2) * 512)
                Hh.dma(tabs[(t + 1) % 2][:], self.ropetab[:, :, tsn].rearrange("f p t -> p f t"), [], [f"tab{(t + 1) % 2}"])
                for sub in range(4):
                    r0 = (t + 1) * 512 + sub * 128
                    Hh.dma(hs[sub][:], h_in[r0:r0 + 128, :], [], [f"hs{sub}"])
            for c in range(4):
                rope_chunk(u, uk, O["aq"] + c * 128, O["aq_r"] + c * 128, 128, tab, tk, 0, 1,
                           [(d["QA"][2 * c, :, ts_], 0, 64), (d["QA"][2 * c + 1, :, ts_], 64, 128)])
                rope_chunk(u, uk, O["aqi"] + c * 128, O["aqi_r"] + c * 128, 128, tab, tk, 2, 3,
                           [(d["QI"][2 * c, :, ts_], 0, 64), (d["QI"][2 * c + 1, :, ts_], 64, 128)])
            o, ok = rope_chunk(u, uk, O["akr"], O["akr_r"], 16, tab, tk, 2, 3, None)
            Hh.copy("pool", akrT[:], o[0:16, :], [ok, "akrT"], ["akrT"])
            bX = fm(u, uk, O["aki"], 64)
            bR = fm(u, uk, O["aki_r"], 64)
            Hh.act(sq[0][0:64, :], self.pb[bX][0:64, :], AF.Square, [f"pb{bX}", "sq0"], ["sq0"])
            bM = pbn()
            Hh.mm(self.pb[bM][0:64, :], ones[0:64, 0:64], sq[0][0:64, :], True, True, ["ones", "sq0"], [f"pb{bM}"])
            Hh.act(rbc[0:64, :], self.pb[bM][0:64, :], AF.Sqrt, [f"pb{bM}", "epsc", "rbc"], ["rbc"], bias=self.epsc[0:64, 0:1], scale=1.0 / 64)
            Hh.recip(rbc2[0:64, :], rbc[0:64, :], ["rbc", "rbc2"], ["rbc2"])
            i = cnt["t"] % 3
            cnt["t"] += 1
            Hh.stt("dve", tmpA[i][0:64, :], self.pb[bX][0:64, :], gki[:, 0:1], tab[0:64, 2, :], ALU.mult, ALU.mult,
                   [f"pb{bX}", "gki", tk, f"tA{i}"], [f"tA{i}"])
            Hh.stt("dve", tmpB[i][0:64, :], self.pb[bR][0:64, :], gki[:, 1:2], tab[0:64, 3, :], ALU.mult, ALU.mult,
                   [f"pb{bR}", "gki", tk, f"tB{i}"], [f"tB{i}"])
            Hh.tt("pool", tmpA[i][0:64, :], tmpA[i][0:64, :], tmpB[i][0:64, :], ALU.add, [f"tA{i}", f"tB{i}"], [f"tA{i}"])
            o, ok = nxt_ob()
            Hh.tt("pool", o[0:64, :], tmpA[i][0:64, :], rbc2[0:64, :], ALU.mult, [f"tA{i}", "rbc2", ok], [ok])
            Hh.dma(d["KI"][:, ts_], o[0:64, :], [ok], [])
            b0 = fm(u, uk, O["ackv"], 128)
            b1 = fm(u, uk, O["ackv"] + 128, 128)
            Hh.act(sq[0][:], self.pb[b0][:], AF.Square, [f"pb{b0}", "sq0"], ["sq0"])
            Hh.act(sq[1][:], self.pb[b1][:], AF.Square, [f"pb{b1}", "sq1"], ["sq1"])
            bM = pbn()
            Hh.mm(self.pb[bM][:], ones[:], sq[0][:], True, False, ["ones", "sq0"], [f"pb{bM}"])
            Hh.mm(self.pb[bM][:], ones[:], sq[1][:], False, True, ["ones", "sq1"], [f"pb{bM}"])
            Hh.act(rbc[:], self.pb[bM][:], AF.Sqrt, [f"pb{bM}", "epsc", "rbc"], ["rbc"], bias=self.epsc[:, 0:1], scale=1.0 / 256)
            Hh.recip(rbc2[:], rbc[:], ["rbc", "rbc2"], ["rbc2"])
            Hh.stt("dve", ckvT[:, 0, :], self.pb[b0][:], gkv[:, 0:1], rbc2[:], ALU.mult, ALU.mult, [f"pb{b0}", "gkv", "rbc2", "ckvT"], ["ckvT"])
            Hh.stt("dve", ckvT[:, 1, :], self.pb[b1][:], gkv[:, 1:2], rbc2[:], ALU.mult, ALU.mult, [f"pb{b1}", "gkv", "rbc2", "ckvT"], ["ckvT"])
            for h in range(8):
                bank = pbn()
                bk = f"pb{bank}"
                Hh.mm(self.pb[bank][0:64, :], wuk[:, 0, h * 64:(h + 1) * 64], ckvT[:, 0, :], True, False, ["wuk", "ckvT"], [bk])
                Hh.mm(self.pb[bank][0:64, :], wuk[:, 1, h * 64:(h + 1) * 64], ckvT[:, 1, :], False, False, ["wuk", "ckvT"], [bk])
                Hh.mm(self.pb[bank][0:64, :], self.ident[0:16, 0:64], akrT[:], False, True, ["ident", "akrT"], [bk])
                o, ok = nxt_ob()
                Hh.copy("act", o[0:64, :], self.pb[bank][0:64, :], [bk, ok], [ok])
                Hh.dma(d["KA"][h, :, ts_], o[0:64, :], [ok], [])
            for sub in range(4):
                bank = pbn()
                bk = f"pb{bank}"
                Hh.mm(self.pb[bank][:], ckvT[:, 0, sub * 128:(sub + 1) * 128], wuv[:, 0, :], True, False, ["ckvT", "wuv"], [bk])
                Hh.mm(self.pb[bank][:], ckvT[:, 1, sub * 128:(sub + 1) * 128], wuv[:, 1, :], False, True, ["ckvT", "wuv"], [bk])
                o, ok = nxt_ob()
                Hh.copy("act", o[:], self.pb[bank][:], [bk, ok], [ok])
                r0 = t * 512 + sub * 128
                Hh.dma(d["VA"][r0:r0 + 128, :], o[:], [ok], [])
            bank = pbn()
            bk = f"pb{bank}"
            for sub in range(4):
                for k in range(8):
                    Hh.mm(self.pb[bank][:, sub * 8:(sub + 1) * 8], u[:, k, sub * 128:(sub + 1) * 128], w[:, k, O["awi"]:O["awi"] + 8],
                          k == 0, k == 7, [uk, "wab"], [bk])
            Hh.ts("dve", wis[:].rearrange("p a b -> p (a b)"), self.pb[bank][:, 0:32], float(8 ** -0.5 * 64 ** -0.5), None, ALU.mult, None,
                  [bk, "wis"], ["wis"])
            Hh.dma(d["WI"][:, t * 4:(t + 1) * 4, :], wis[:], ["wis"], [])
            for c in range(4):
                for nm, dst, scl in (("bq", d["BQ"], 0.125), ("bk", d["BK"], 1.0)):
                    bank = fm(u, uk, O[nm] + c * 128, 128)
                    o, ok = nxt_ob()
                    Hh.act(o[:], self.pb[bank][:], AF.Copy, [f"pb{bank}", ok], [ok], scale=scl)
                    Hh.dma(dst[2 * c, 0:64, ts_], o[0:64, :], [ok], [])
                    Hh.dma(dst[2 * c + 1, 0:64, ts_], o[64:128, :], [ok], [])
            for sub in range(4):
                bank = pbn()
                bk = f"pb{bank}"
                for k in range(8):
                    Hh.mm(self.pb[bank][:], u[:, k, sub * 128:(sub + 1) * 128], w[:, k, O["bv"]:O["bv"] + 512], k == 0, k == 7, [uk, "wab"], [bk])
                o, ok = nxt_ob()
                Hh.copy("act", o[:], self.pb[bank][:], [bk, ok], [ok])
                r0 = t * 512 + sub * 128
                Hh.dma(d["VB"][r0:r0 + 128, :], o[:], [ok], [])
            bank = fm(u, uk, O["bf"], 8)
            i = cnt["t"] % 3
            cnt["t"] += 1
            Hh.copy("dve", tmpA[i][0:8, :], self.pb[bank][0:8, :], [f"pb{bank}", f"tA{i}"], [f"tA{i}"])
            Hh.dma(d["BFT"][:, ts_], tmpA[i][0:8, :], [f"tA{i}"], [])
        P.flush()


K.phase_proj_ab = phase_proj_ab


def load_const_bf16(self, es, name, dram, shape):
    Hh = self.Hh
    t = self.sbuf(es, name, shape, BF16)
    n = int(np.prod(shape[1:]))
    flat_t = t[:] if len(shape) == 2 else t[:].rearrange("p a b -> p (a b)")
    flat_d = dram if len(shape) == 2 else dram.rearrange("p a b -> p (a b)")
    for c0 in range(0, n, 2048):
        cw = min(2048, n - c0)
        s = self.stg[self.stgi % 2]
        sk = f"stg{self.stgi % 2}"
        self.stgi += 1
        Hh.dma(s[0:shape[0], 0:cw], flat_d[:, c0:c0 + cw], [], [sk])
        Hh.copy("pool", flat_t[:, c0:c0 + cw], s[0:shape[0], 0:cw], [sk, name], [name])
    return t


K.load_const_bf16 = load_const_bf16


def phase_decay(self, layer):
    nc, P, Hh, S = self.nc, self.P, self.Hh, self.S
    d = self.ab[layer // 2]
    NT = self.NT
    with ExitStack() as es:
        sb = lambda name, shape, dt: self.sbuf(es, name, shape, dt)
        xt = sb("xt", [8, S], F32)
        lt = sb("lt", [8, S], F32)
        cum = sb("cum", [8, S], F32)
        one = sb("one", [8, S], F32)
        t32 = sb("t32", [8, S], F32)
        hi = sb("hi", [8, S], BF16)
        lo = sb("lo", [8, S], BF16)
        lo2 = sb("lo2", [8, S], BF16)
        oneb = sb("oneb", [8, S], BF16)
        bfg = sb("bfg", [8, 2], F32)
        onec = sb("onec", [8, 1], F32)
        ncum = sb("ncum", [128, NT, 8], F32)
        Hh.dma(xt[:], d["BFT"], [], ["xt"])
        Hh.dma(bfg[:, 0:1], d["b_forget"], [], ["bfg"])
        Hh.memset("pool", one[:], 1.0, ["one"])
        Hh.memset("pool", oneb[:], 1.0, ["oneb"])
        Hh.memset("pool", onec[:], 1.0, ["onec"])
        Hh.ts("dve", bfg[:, 1:2], bfg[:, 0:1], -1.0, None, ALU.mult, None, ["bfg"], ["bfg2"])
        Hh.act(lt[:], xt[:], AF.Exp, ["xt", "bfg2", "lt"], ["lt"], bias=bfg[:, 1:2], scale=-1.0)
        Hh.act(xt[:], lt[:], AF.Ln, ["lt", "onec", "xt"], ["xt"], bias=onec[:, 0:1], scale=1.0)
        P.op("dve", lambda e: e.tensor_tensor_scan(out=cum[:], data0=one[:], data1=xt[:], initial=0.0,
                                                   op0=ALU.mult, op1=ALU.subtract), ["one", "xt"], ["cum"])
        Hh.copy("dve", hi[:], cum[:], ["cum"], ["hi"])
        Hh.copy("dve", t32[:], hi[:], ["hi"], ["t32"])
        Hh.tt("dve", t32[:], cum[:], t32[:], ALU.subtract, ["cum", "t32"], ["t32"])
        Hh.copy("dve", lo[:], t32[:], ["t32"], ["lo"])
        Hh.copy("dve", lt[:], lo[:], ["lo", "lt"], ["lt"])
        Hh.tt("dve", t32[:], t32[:], lt[:], ALU.subtract, ["t32", "lt"], ["t32"])
        Hh.copy("dve", lo2[:], t32[:], ["t32"], ["lo2"])
        Hh.dma(d["BQ"][:, 64, :], hi[:], ["hi"], [])
        Hh.dma(d["BQ"][:, 65, :], lo[:], ["lo"], [])
        Hh.dma(d["BK"][:, 64, :], oneb[:], ["oneb"], [])
        Hh.dma(d["BK"][:, 65, :], oneb[:], ["oneb"], [])
        pbn = rr([0, 1, 2, 3])
        for g in range(0, NT, 16):
            bank = pbn()
            bk = f"pb{bank}"
            n = min(16, NT - g)
            for q in range(n):
                tsl = slice((g + q) * 128, (g + q + 1) * 128)
                for ti, (src, sk) in enumerate(((hi, "hi"), (lo, "lo"), (lo2, "lo2"))):
                    Hh.mm(self.pb[bank][:, q * 8:(q + 1) * 8], src[0:8, tsl], self.ident[0:8, 0:8], ti == 0, ti == 2, [sk, "ident"], [bk])
            Hh.ts("dve", ncum[:, g:g + n, :].rearrange("p a b -> p (a b)"), self.pb[bank][:, 0:n * 8], -1.0, None, ALU.mult, None,
                  [bk, "ncum"], ["ncum"])
        Hh.dma(d["NCUM"], ncum[:], ["ncum"], [])
        P.flush()


K.phase_decay = phase_decay


def phase_idx(self, layer):
    nc, P, Hh, S = self.nc, self.P, self.Hh, self.S
    d = self.ab[layer // 2]
    NT = self.NT
    NKEEP = min(256, S // 4)
    NIT = 20
    with ExitStack() as es:
        sb = lambda name, shape, dt: self.sbuf(es, name, shape, dt)
        ki = sb("ki", [64, S], BF16)
        wi = sb("wi", [128, NT, 8], F32)
        caus = sb("caus", [128, 128], F32)
        pow2 = sb("pow2", [128, NIT], F32)
        kap = sb("kap", [128, 1], F32)
        Hh.dma(ki[:], d["KI"], [], ["ki"])
        Hh.dma(wi[:], d["WI"], [], ["wi"])
        Hh.dma(caus[:], self.c_caus, [], ["caus"])
        Hh.dma(pow2[:], self.c_pow2[:, 0:NIT], [], ["pow2"])
        Hh.memset("dve", kap[:], float(NKEEP) - 0.5, ["kap"])
        NB = 4
        qi = [sb(f"qi{i}", [64, 8, 128], BF16) for i in range(NB)]
        I = [sb(f"I{i}", [128, S], F32) for i in range(NB)]
        mb = [sb(f"mb{i}", [128, S], BF16) for i in range(NB)]
        dg = [sb(f"dg{i}", [128, 8, 128], BF16) for i in range(NB)]
        junk = [sb(f"junkb{i}", [128, S], BF16) for i in range(2)]
        r = [sb(f"r{i}", [128, 512], BF16) for i in range(4)]
        st = [dict(hi=sb(f"hi{i}", [128, 1], F32), lo=sb(f"lo{i}", [128, 1], F32), w0=sb(f"w0{i}", [128, 1], F32),
                   steps=sb(f"steps{i}", [128, NIT], F32), nst=sb(f"nst{i}", [128, NIT], F32), mid=sb(f"mid{i}", [128, 1], F32),
                   cnt=sb(f"cnt{i}", [128, 1], F32), g=sb(f"g{i}", [128, 1], F32)) for i in range(NB)]
        pbn = rr([0, 1, 2, 3, 4, 5])
        ci = [0]
        rcnt = [0]

        def indexer(i):
            p = i % NB
            Ik, qk = f"I{p}", f"qi{p}"
            L = (i + 1) * 128
            Hh.dma(qi[p][:], d["QI"][:, :, i * 128:(i + 1) * 128].rearrange("h d t -> d h t"), [], [qk])
            dgp = dg[p]
            for h in range(8):
                Hh.ts("dve", dgp[:, h, :], self.ident[:], wi[:, i, h:h + 1], None, ALU.mult, None, ["ident", "wi", f"dg{p}"], [f"dg{p}"])
            items = []
            for c in range((L + 511) // 512):
                wd = min(512, L - c * 512)
                ib = 6 + (ci[0] % 2)
                ci[0] += 1
                for h in range(8):
                    items.append((c, h, wd, ib))
            pend = {}

            def pre(it):
                c, h, wd, ib = it
                cs = slice(c * 512, c * 512 + wd)
                bank = pbn()
                bk = f"pb{bank}"
                Hh.mm(self.pb[bank][:, 0:wd], qi[p][:, h, :], ki[:, cs], True, True, [qk, "ki"], [bk])
                n = rcnt[0]
                rcnt[0] += 1
                rb, rk = r[n % 4], f"r{n % 4}"
                Hh.act(rb[:, 0:wd], self.pb[bank][:, 0:wd], AF.Relu, [bk, rk], [rk])
                pend[it] = (rb, rk)

            def post(it):
                c, h, wd, ib = it
                cs = slice(c * 512, c * 512 + wd)
                rb, rk = pend.pop(it)
                Hh.mm(self.pb[ib][:, 0:wd], dgp[:, h, :], rb[:, 0:wd], h == 0, h == 7, [f"dg{p}", rk], [f"pb{ib}"])
                if h == 7:
                    Hh.copy("act", I[p][:, cs], self.pb[ib][:, 0:wd], [f"pb{ib}", Ik], [Ik])

            for k in range(min(2, len(items))):
                pre(items[k])
            for k, it in enumerate(items):
                if k + 2 < len(items):
                    pre(items[k + 2])
                post(it)

        def bisect_steps(i, jn):
            p = i % NB
            Ik, mk = f"I{p}", f"mb{p}"
            L = (i + 1) * 128
            s = st[p]
            sk = f"st{p}"
            jb, jk = junk[jn], f"junkb{jn}"
            steps = []
            steps.append(lambda: Hh.tt("dve", I[p][:, i * 128:L], I[p][:, i * 128:L], caus[:], ALU.add, [Ik, "caus"], [Ik]))
            if i * 128 < NKEEP:
                steps.append(lambda: Hh.memset("dve", s["lo"][:], -1e29, [sk]))
            else:
                steps.append(lambda: P.op("dve", lambda e: e.reduce_max(out=s["hi"][:], in_=I[p][:, 0:L], axis=AX.X), [Ik, sk], [sk]))
                steps.append(lambda: P.op("dve", lambda e: e.tensor_reduce(out=s["lo"][:], in_=I[p][:, 0:i * 128], axis=AX.X, op=ALU.min), [Ik, sk], [sk]))
                steps.append(lambda: Hh.tt("dve", s["w0"][:], s["hi"][:], s["lo"][:], ALU.subtract, [sk], [sk]))
                steps.append(lambda: Hh.ts("dve", s["steps"][:], pow2[:], s["w0"][:, 0:1], None, ALU.mult, None, ["pow2", sk], [sk]))
                steps.append(lambda: Hh.ts("dve", s["nst"][:], s["steps"][:], -1.0, None, ALU.mult, None, [sk], [sk]))
                steps.append(lambda: Hh.tt("dve", s["mid"][:], s["lo"][:], s["steps"][:, 0:1], ALU.add, [sk], [sk]))
                for it in range(NIT):
                    steps.append(lambda: Hh.ts("dve", jb[:, 0:L], I[p][:, 0:L], s["mid"][:, 0:1], None, ALU.is_ge, ALU.add, [Ik, sk, jk], [jk, sk],
                                               accum_out=s["cnt"][:, 0:1]))
                    if it + 1 < NIT:
                        steps.append(lambda it=it: Hh.stt("dve", s["g"][:], s["cnt"][:], kap[:, 0:1], s["steps"][:, it:it + 1], ALU.is_ge, ALU.mult,
                                                          [sk, "kap"], [sk]))
                        steps.append(lambda it=it: Hh.stt("dve", s["mid"][:], s["g"][:], s["nst"][:, it + 1:it + 2], s["mid"][:], ALU.add, ALU.add,
                                                          [sk], [sk]))
                    else:
                        steps.append(lambda it=it: Hh.stt("dve", s["g"][:], s["cnt"][:], kap[:, 0:1], s["steps"][:, it:it + 1], ALU.is_ge, ALU.mult,
                                                          [sk, "kap"], [sk]))
                        steps.append(lambda it=it: Hh.stt("dve", s["lo"][:], s["g"][:], s["nst"][:, it:it + 1], s["mid"][:], ALU.add, ALU.add,
                                                          [sk], [sk]))
            steps.append(lambda: Hh.ts("dve", mb[p][:, 0:L], I[p][:, 0:L], s["lo"][:, 0:1], None, ALU.is_lt, None, [Ik, sk, mk], [mk]))
            steps.append(lambda: Hh.dma(d["MB"][i * 128:(i + 1) * 128, 0:L], mb[p][:, 0:L], [mk], []))
            return steps

        npair = (NT + 1) // 2
        pairs = [[t for t in (2 * m, 2 * m + 1) if t < NT] for m in range(npair)]
        for t in pairs[0]:
            indexer(t)
        for m in range(npair):
            if m + 1 < npair:
                for t in pairs[m + 1]:
                    indexer(t)
            lists = [bisect_steps(t, n) for n, t in enumerate(pairs[m])]
            for k in range(max(len(l) for l in lists)):
                for l in lists:
                    if k < len(l):
                        l[k]()
        P.flush()


K.phase_idx = phase_idx


def phase_attn_ab(self, layer):
    nc, P, Hh, S = self.nc, self.P, self.Hh, self.S
    d = self.ab[layer // 2]
    NT = self.NT
    with ExitStack() as es:
        sb = lambda name, shape, dt: self.sbuf(es, name, shape, dt)
        self.stg = [sb("stg0", [128, 2048], F32), sb("stg1", [128, 2048], F32)]
        self.stgi = 0
        ncum = sb("ncum", [128, NT, 8], F32)
        Hh.dma(ncum[:], d["NCUM"], [], ["kbias"])
        cm = self.load_const_bf16(es, "cm", self.c_cm, [128, 4, 512])
        negi = self.load_const_bf16(es, "negi", self.c_negi, [128, 128])
        heads = []
        for h in range(8):
            heads.append(dict(q=d["BQ"][h], k=d["BK"][h], Kd=66, v=d["VB"][:, h * 64:(h + 1) * 64], ocol=512 + h * 64,
                              kbias=(lambda kt, h=h: ncum[:, kt, h:h + 1]),
                              kts=(lambda j: list(range(4 * j + 4))),
                              terms=(lambda j, kt, par: [(self.ident[:], cm[:, kt - 4 * j, :], ("ident", "cm"))] if kt >= 4 * j else [])))
        self.attention(es, heads, 512, d["OCAT"], S, tagp="fox")
        P.flush()
    with ExitStack() as es:
        sb = lambda name, shape, dt: self.sbuf(es, name, shape, dt)
        self.stg = [sb("stg0", [128, 2048], F32), sb("stg1", [128, 2048], F32)]
        self.stgi = 0
        negi = self.load_const_bf16(es, "negi", self.c_negi, [128, 128])
        mbs = [sb(f"mbs{i}", [128, S], BF16) for i in range(3)]

        def unit_load(hi, j, par):
            L = (j + 1) * 128
            Hh.dma(mbs[par][:, 0:L], d["MB"][j * 128:(j + 1) * 128, 0:L], [], [f"mbs{par}"])

        heads = []
        for h in range(8):
            heads.append(dict(q=d["QA"][h], k=d["KA"][h], Kd=64, v=d["VA"][:, h * 64:(h + 1) * 64], ocol=h * 64,
                              kts=(lambda j: list(range(j + 1))),
                              terms=(lambda j, kt, par: [(mbs[par][:, kt * 128:(kt + 1) * 128], negi[:], (f"mbs{par}", "negi"))])))
        self.attention(es, heads, 128, d["OCAT"], S, unit_load=unit_load, tagp="dsa")
        P.flush()


K.phase_attn_ab = phase_attn_ab


def phase_out(self, srcs, wout_d, h_in, h_out):
    nc, P, Hh, S = self.nc, self.P, self.Hh, self.S
    with ExitStack() as es:
        sb = lambda name, shape, dt: self.sbuf(es, name, shape, dt)
        self.stg = [sb("stg0", [128, 2048], F32), sb("stg1", [128, 2048], F32)]
        self.stgi = 0
        w = self.load_weight_bf16(es, "wout", wout_d, 8, D)
        ob = [[sb(f"o{i}_{s}", [128, D], BF16) for s in range(len(srcs))] for i in range(3)]
        hs = [sb(f"hs{i}", [128, D], F32) for i in range(3)]
        oT = [sb(f"oT{i}", [128, 8, 128], BF16) for i in range(3)]
        pbn = rr([0, 1, 2, 3, 4, 5, 6, 7])

        def out_loads(t_):
            p_ = t_ % 3
            rs_ = slice(t_ * 128, (t_ + 1) * 128)
            for s_, src_ in enumerate(srcs):
                Hh.dma(ob[p_][s_][:], src_[rs_, :], [], [f"o{p_}_{s_}"])
            Hh.dma(hs[p_][:], h_in[rs_, :], [], [f"hs{p_}"])

        for t in range(self.NT):
            p = t % 3
            rs = slice(t * 128, (t + 1) * 128)
            if t == 0:
                out_loads(0)
            if t + 1 < self.NT:
                out_loads(t + 1)
            for s in range(1, len(srcs)):
                Hh.tt("pool", ob[p][0][:], ob[p][0][:], ob[p][s][:], ALU.add, [f"o{p}_0", f"o{p}_{s}"], [f"o{p}_0"])
            for half in range(2):
                bank = pbn()
                bk = f"pb{bank}"
                for q in range(4):
                    k = half * 4 + q
                    Hh.mm(self.pb[bank][:, q * 128:(q + 1) * 128], ob[p][0][:, k * 128:(k + 1) * 128], self.ident[:], True, True,
                          [f"o{p}_0", "ident"], [bk])
                Hh.copy("act", oT[p][:, half * 4:half * 4 + 4, :], self.pb[bank][:].rearrange("p (q t) -> p q t", q=4), [bk, f"oT{p}"], [f"oT{p}"])
            for half in range(2):
                bank = pbn()
                bk = f"pb{bank}"
                for k in range(8):
                    Hh.mm(self.pb[bank][:], oT[p][:, k, :], w[:, k, half * 512:(half + 1) * 512], k == 0, k == 7, [f"oT{p}", "wout"], [bk])
                Hh.tt("dve", hs[p][:, half * 512:(half + 1) * 512], hs[p][:, half * 512:(half + 1) * 512], self.pb[bank][:], ALU.add,
                      [bk, f"hs{p}"], [f"hs{p}"])
            Hh.dma(h_out[rs, :], hs[p][:], [f"hs{p}"], [])
        P.flush()


K.phase_out = phase_out


def layer_ab(self, layer, h_in, h_out):
    d = self.ab[layer // 2]
    self.phase_proj_ab(layer, h_in)
    self.phase_decay(layer)
    self.phase_idx(layer)
    self.phase_attn_ab(layer)
    self.phase_out([d["OCAT"]], d["w_out"], h_in, h_out)


K.layer_ab = layer_ab


C_COLS = dict(q=(0, 1024), kc=(1024, 1152), vc=(1152, 1280), ks=(1280, 1408), vs=(1408, 1536), kw=(1536, 1664),
              vw=(1664, 1792), gl=(1792, 1840))


def c_weight_layout(w_in):
    g = lambda n: w_in[:, C_COLS[n][0]:C_COLS[n][1]]
    blocks = [("q", g("q")), ("q_r", rot_cols(g("q"), 16, 64)), ("kc", g("kc")), ("kc_r", rot_cols(g("kc"), 2, 64)),
              ("ks", g("ks")), ("ks_r", rot_cols(g("ks"), 2, 64)), ("kw", g("kw")), ("kw_r", rot_cols(g("kw"), 2, 64)),
              ("vc", g("vc")), ("vs", g("vs")), ("vw", g("vw")), ("gl", g("gl"))]
    offs, c = {}, 0
    for n, b in blocks:
        offs[n] = c
        c += b.shape[1]
    return np.ascontiguousarray(np.concatenate([b for _, b in blocks], axis=1)), offs, c


C_OFFS = c_weight_layout(np.zeros((1, 1840), np.float32))[1]
C_NC = c_weight_layout(np.zeros((1, 1840), np.float32))[2]


def decl_c(self, j):
    S, NT = self.S, self.NT
    d = {}
    d["wc"] = self.din(f"c_w_{j}", [D, C_NC])
    d["g_attn"] = self.din(f"c_g_{j}", [128, 8])
    d["b_gate"] = self.din(f"c_bg_{j}", [128, 48])
    d["pe_k"] = self.din(f"c_pek_{j}", [128, 2048])
    d["pe_v"] = self.din(f"c_pev_{j}", [128, 2048])
    d["wk1"] = self.din(f"c_wk1_{j}", [2048, 128])
    d["wk2"] = self.din(f"c_wk2_{j}", [128, 64])
    d["wv1"] = self.din(f"c_wv1_{j}", [2048, 128])
    d["wv2"] = self.din(f"c_wv2_{j}", [128, 64])
    d["w_out"] = self.din(f"c_wout_{j}", [D, D])
    for nm, shp, dt in (("QC", [16, 64, S], BF16), ("KCg", [2, S, 64], BF16), ("VCg", [2, S, 64], BF16),
                        ("KS", [2, 64, S], BF16), ("VS", [S, 128], BF16), ("KW", [2, 64, S], BF16), ("VW", [S, 128], BF16),
                        ("G", [128, NT, 48], F32), ("KCMP", [2, 64, 256], BF16), ("VCMP", [2, 256, 64], BF16),
                        ("SBT", [2, 64, S], BF16), ("EBF", [64, S], BF16), ("OC", [S, D], BF16), ("OS", [S, D], BF16), ("OW", [S, D], BF16)):
        d[nm] = self.dscr(f"{nm}_{j}", shp, dt)
    self.cl[j] = d
    if not hasattr(self, "c_cmpm"):
        self.c_cmpm = self.din("c_cmpm", [128, 5, 512])
        self.c_wm = self.din("c_wm", [128, 4, 512])
        self.c_E = self.din("c_E", [64, S])
        self.c_F = self.din("c_F", [128, 503])
        self.c_PA = self.din("c_PA", [128, 127])
        self.c_PB = self.din("c_PB", [128, 127])


K.decl_c = decl_c


def host_inputs_c(m, S, inp, l):
    j = l // 2
    m[f"c_w_{j}"] = c_weight_layout(inp["c_w_in"][j])[0]
    m[f"c_g_{j}"] = np.ascontiguousarray(inp["attn_norm"][l].reshape(8, 128).T)
    m[f"c_bg_{j}"] = np.ascontiguousarray(np.broadcast_to(inp["c_b_gate"][j][None, :], (128, 48)))
    m[f"c_pek_{j}"] = np.ascontiguousarray(np.broadcast_to(inp["c_pe_k"][j].reshape(1, 2048), (128, 2048)))
    m[f"c_pev_{j}"] = np.ascontiguousarray(np.broadcast_to(inp["c_pe_v"][j].reshape(1, 2048), (128, 2048)))
    m[f"c_wk1_{j}"] = np.ascontiguousarray(inp["c_cmp_k_w1"][j])
    m[f"c_wk2_{j}"] = np.ascontiguousarray(inp["c_cmp_k_w2"][j])
    m[f"c_wv1_{j}"] = np.ascontiguousarray(inp["c_cmp_v_w1"][j])
    m[f"c_wv2_{j}"] = np.ascontiguousarray(inp["c_cmp_v_w2"][j])
    m[f"c_wout_{j}"] = np.ascontiguousarray(inp["c_w_out"][j])
    if "c_cmpm" not in m:
        p = np.arange(128)[:, None, None]
        c = np.arange(512)[None, None, :]
        mm = np.arange(5)[None, :, None]
        m["c_cmpm"] = np.where(16 * p + 31 - 512 * mm <= c, 0.0, NEG).astype(np.float32)
        r = np.arange(4)[None, :, None]
        m["c_wm"] = np.where(128 * r + p > c, 0.0, NEG).astype(np.float32)
        m["c_E"] = (np.arange(S)[None, :] // 64 == np.arange(64)[:, None]).astype(np.float32)
        mp = np.arange(503)[None, :] - 248
        pp = np.arange(128)[:, None]
        m["c_F"] = np.where(16 * mp + 31 <= pp, 0.0, NEG).astype(np.float32)
        jp = np.arange(127)[None, :] - 63
        hi = (pp >= 64)
        PA = np.ones((128, 127), np.float32)
        PB = np.zeros((128, 127), np.float32)
        f_m1 = (jp == -1) & (~hi)
        f_0 = (jp == 0)
        f_1 = (jp == 1) & hi
        inv = ((jp == 1) & (~hi)) | (jp >= 2)
        PA[f_m1 | f_0 | f_1 | inv] = 0.0
        PB[np.broadcast_to(f_m1, PB.shape)] = 10000.0
        PB[np.broadcast_to(f_0, PB.shape)] = 10001.0
        PB[np.broadcast_to(f_1, PB.shape)] = 10002.0
        PB[np.broadcast_to(inv, PB.shape)] = -1e30
        m["c_PA"] = PA
        m["c_PB"] = PB


def phase_proj_c(self, layer, h_in):
    nc, P, Hh, S = self.nc, self.P, self.Hh, self.S
    d = self.cl[layer // 2]
    O = C_OFFS
    NQ = S // 512
    with ExitStack() as es:
        sb = lambda name, shape, dt: self.sbuf(es, name, shape, dt)
        self.stg = [sb("stg0", [128, 2048], F32), sb("stg1", [128, 2048], F32)]
        self.stgi = 0
        gain = sb("gain", [128, 8], F32)
        Hh.dma(gain[:], d["g_attn"], [], ["gain"])
        w = self.load_weight_bf16(es, "wc", d["wc"], 8, C_NC, gain=gain)
        bg = sb("bg", [128, 48], F32)
        Hh.dma(bg[:], d["b_gate"], [], ["bg"])
        hs = [sb(f"hs{i}", [128, D], F32) for i in range(4)]
        uT = [sb(f"uT{i}", [128, 8, 512], BF16) for i in range(2)]
        scr = [dict(ss=sb(f"ss{i}", [128, 1], F32), rstd=sb(f"rstd{i}", [128, 2], F32),
                    ub=sb(f"ub{i}", [128, D], BF16), junk=sb(f"junk{i}", [128, D], F32)) for i in range(2)]
        tabs = [sb(f"tab{i}", [128, 4, 512], F32) for i in range(2)]
        tmpA = [sb(f"tA{i}", [128, 512], F32) for i in range(3)]
        tmpB = [sb(f"tB{i}", [128, 512], F32) for i in range(3)]
        obf = [sb(f"ob{i}", [128, 512], BF16) for i in range(4)]
        gs = sb("gs", [128, 4, 48], F32)
        pbn = rr([0, 1, 2, 3, 4, 5, 6, 7])
        cnt = dict(t=0, o=0)

        def fm(u, uk, c0, M):
            bank = pbn()
            for k in range(8):
                Hh.mm(self.pb[bank][0:M, :], w[:, k, c0:c0 + M], u[:, k, :], k == 0, k == 7, ["wc", uk], [f"pb{bank}"])
            return bank

        def nxt_ob():
            i = cnt["o"] % 4
            cnt["o"] += 1
            return obf[i], f"ob{i}"

        def rope_chunk(u, uk, cX, cR, tab, tk, ci, si):
            bX = fm(u, uk, cX, 128)
            bR = fm(u, uk, cR, 128)
            i = cnt["t"] % 3
            cnt["t"] += 1
            Hh.tt("dve", tmpA[i][:], self.pb[bX][:], tab[:, ci, :], ALU.mult, [f"pb{bX}", tk, f"tA{i}"], [f"tA{i}"])
            Hh.tt("dve", tmpB[i][:], self.pb[bR][:], tab[:, si, :], ALU.mult, [f"pb{bR}", tk, f"tB{i}"], [f"tB{i}"])
            o, ok = nxt_ob()
            Hh.tt("pool", o[:], tmpA[i][:], tmpB[i][:], ALU.add, [f"tA{i}", f"tB{i}", ok], [ok])
            return o, ok

        for t in range(NQ):
            u, uk = uT[t % 2], f"uT{t % 2}"
            tab, tk = tabs[t % 2], f"tab{t % 2}"
            ts_ = slice(t * 512, (t + 1) * 512)
            if t == 0:
                Hh.dma(tab[:], self.ropetab[:, :, ts_].rearrange("f p t -> p f t"), [], [tk])
                for sub in range(4):
                    Hh.dma(hs[sub][:], h_in[sub * 128:(sub + 1) * 128, :], [], [f"hs{sub}"])
            for sub in range(4):
                sc = scr[sub % 2]
                sc["hkey"] = f"hs{sub}"
                sc["uTkey"] = uk
                self.rmsnorm_T(hs[sub][:], sub, u, sub * 128, None, f"p{sub % 2}", pbn, sc)
            if t + 1 < NQ:
                tsn = slice((t + 1) * 512, (t + 2) * 512)
                Hh.dma(tabs[(t + 1) % 2][:], self.ropetab[:, :, tsn].rearrange("f p t -> p f t"), [], [f"tab{(t + 1) % 2}"])
                for sub in range(4):
                    r0 = (t + 1) * 512 + sub * 128
                    Hh.dma(hs[sub][:], h_in[r0:r0 + 128, :], [], [f"hs{sub}"])
            for c in range(8):
                o, ok = rope_chunk(u, uk, O["q"] + c * 128, O["q_r"] + c * 128, tab, tk, 0, 1)
                Hh.dma(d["QC"][2 * c, :, ts_], o[0:64, :], [ok], [])
                Hh.dma(d["QC"][2 * c + 1, :, ts_], o[64:128, :], [ok], [])
            for nm, dst in (("ks", d["KS"]), ("kw", d["KW"])):
                o, ok = rope_chunk(u, uk, O[nm], O[nm + "_r"], tab, tk, 2, 3)
                Hh.dma(dst[0, :, ts_], o[0:64, :], [ok], [])
                Hh.dma(dst[1, :, ts_], o[64:128, :], [ok], [])
            o, ok = rope_chunk(u, uk, O["kc"], O["kc_r"], tab, tk, 2, 3)
            bank = pbn()
            bk = f"pb{bank}"
            for sub in range(4):
                Hh.mm(self.pb[bank][:, sub * 128:(sub + 1) * 128], o[:, sub * 128:(sub + 1) * 128], self.ident[:], True, True, [ok, "ident"], [bk])
            o2, ok2 = nxt_ob()
            Hh.copy("act", o2[:], self.pb[bank][:], [bk, ok2], [ok2])
            for sub in range(4):
                r0 = t * 512 + sub * 128
                for g in range(2):
                    Hh.dma(d["KCg"][g, r0:r0 + 128, :], o2[:, sub * 128 + g * 64:sub * 128 + g * 64 + 64], [ok2], [])
            for sub in range(4):
                r0 = t * 512 + sub * 128
                bank = pbn()
                bk = f"pb{bank}"
                for k in range(8):
                    Hh.mm(self.pb[bank][:, 0:432], u[:, k, sub * 128:(sub + 1) * 128], w[:, k, O["vc"]:O["vc"] + 432], k == 0, k == 7, [uk, "wc"], [bk])
                o, ok = nxt_ob()
                Hh.copy("act", o[:, 0:384], self.pb[bank][:, 0:384], [bk, ok], [ok])
                for g in range(2):
                    Hh.dma(d["VCg"][g, r0:r0 + 128, :], o[:, g * 64:(g + 1) * 64], [ok], [])
                Hh.dma(d["VS"][r0:r0 + 128, :], o[:, 128:256], [ok], [])
                Hh.dma(d["VW"][r0:r0 + 128, :], o[:, 256:384], [ok], [])
                Hh.tt("dve", gs[:, sub, :], self.pb[bank][:, 384:432], bg[:], ALU.add, [bk, "bg", "gs"], ["gs"])
            Hh.act(gs[:].rearrange("p a b -> p (a b)"), gs[:].rearrange("p a b -> p (a b)"), AF.Sigmoid, ["gs"], ["gs"])
            Hh.dma(d["G"][:, t * 4:(t + 1) * 4, :], gs[:], ["gs"], [])
        P.flush()


K.phase_proj_c = phase_proj_c


def phase_cmp(self, layer):
    nc, P, Hh, S = self.nc, self.P, self.Hh, self.S
    d = self.cl[layer // 2]
    NCMP = S // 16 - 1
    ntl = [(0, min(128, NCMP))] + ([(128, NCMP - 128)] if NCMP > 128 else [])
    with ExitStack() as es:
        sb = lambda name, shape, dt: self.sbuf(es, name, shape, dt)
        self.stg = [sb("stg0", [128, 2048], F32), sb("stg1", [128, 2048], F32)]
        self.stgi = 0
        w1 = {"k": self.load_weight_bf16(es, "wk1", d["wk1"], 16, 128), "v": self.load_weight_bf16(es, "wv1", d["wv1"], 16, 128)}
        w2 = {"k": self.load_weight_bf16(es, "wk2", d["wk2"], 1, 64), "v": self.load_weight_bf16(es, "wv2", d["wv2"], 1, 64)}
        pe = {"k": sb("pek", [128, 2048], F32), "v": sb("pev", [128, 2048], F32)}
        Hh.dma(pe["k"][:], d["pe_k"], [], ["pek"])
        Hh.dma(pe["v"][:], d["pe_v"], [], ["pev"])
        X = [sb(f"X{i}", [128, 2048], BF16) for i in range(2)]
        Xp = [sb(f"Xp{i}", [128, 2048], BF16) for i in range(2)]
        XT = sb("XT", [128, 16, 256], BF16)
        hx = sb("hx", [128, 256], F32)
        t1 = sb("t1", [128, 256], F32)
        t2 = sb("t2", [128, 256], F32)
        gl = sb("gl", [128, 256], BF16)
        oc = sb("oc", [128, 256], BF16)
        pbn = rr([0, 1, 2, 3, 4, 5, 6, 7])
        xi = 0
        for g in range(2):
            for kv, src in (("k", d["KCg"]), ("v", d["VCg"])):
                V = src[g].rearrange("(a b) e -> a (b e)", b=16)
                for (n0, nr) in ntl:
                    p = xi % 2
                    xi += 1
                    Hh.dma(X[p][0:nr, 0:1024], V[n0:n0 + nr, :], [], [f"X{p}"])
                    Hh.dma(X[p][0:nr, 1024:2048], V[n0 + 1:n0 + 1 + nr, :], [], [f"X{p}"])
                    Hh.tt("pool", Xp[p][0:nr, :], X[p][0:nr, :], pe[kv][0:nr, :], ALU.add, [f"X{p}", "pe" + kv, f"Xp{p}"], [f"Xp{p}"])
                    for q4 in range(4):
                        bank = pbn()
                        bk = f"pb{bank}"
                        for q in range(4):
                            c = q4 * 4 + q
                            Hh.mm(self.pb[bank][:, q * 128:q * 128 + nr], Xp[p][0:nr, c * 128:(c + 1) * 128], self.ident[0:nr, 0:nr], True, True,
                                  [f"Xp{p}", "ident"], [bk])
                        Hh.copy("act" if q4 % 2 else "dve", XT[:, q4 * 4:q4 * 4 + 4, n0:n0 + nr],
                                self.pb[bank][:].rearrange("p (q t) -> p q t", q=4)[:, :, 0:nr], [bk, "XT"], ["XT"])
                bank = pbn()
                bk = f"pb{bank}"
                for c in range(16):
                    Hh.mm(self.pb[bank][:, 0:NCMP], w1[kv][:, c, :], XT[:, c, 0:NCMP], c == 0, c == 15, ["w" + kv + "1", "XT"], [bk])
                N = NCMP
                Hh.copy("dve", hx[:, 0:N], self.pb[bank][:, 0:N], [bk, "hx"], ["hx"])
                Hh.tt("dve", t1[:, 0:N], hx[:, 0:N], hx[:, 0:N], ALU.mult, ["hx", "t1"], ["t1"])
                Hh.ts("dve", t1[:, 0:N], t1[:, 0:N], 0.044715, 1.0, ALU.mult, ALU.add, ["t1"], ["t1"])
                Hh.tt("dve", t1[:, 0:N], t1[:, 0:N], hx[:, 0:N], ALU.mult, ["t1", "hx"], ["t1"])
                Hh.act(t2[:, 0:N], t1[:, 0:N], AF.Tanh, ["t1", "t2"], ["t2"], scale=0.7978845608028654)
                Hh.stt("dve", t2[:, 0:N], t2[:, 0:N], 1.0, hx[:, 0:N], ALU.add, ALU.mult, ["t2", "hx"], ["t2"])
                Hh.ts("dve", gl[:, 0:N], t2[:, 0:N], 0.5, None, ALU.mult, None, ["t2", "gl"], ["gl"])
                if kv == "k":
                    bank = pbn()
                    bk = f"pb{bank}"
                    Hh.mm(self.pb[bank][0:64, 0:N], w2["k"][:, 0, :], gl[:, 0:N], True, True, ["wk2", "gl"], [bk])
                    Hh.copy("act", oc[0:64, 0:N], self.pb[bank][0:64, 0:N], [bk, "oc"], ["oc"])
                    Hh.dma(d["KCMP"][g, :, 0:N], oc[0:64, 0:N], ["oc"], [])
                else:
                    for (n0, nr) in ntl:
                        bank = pbn()
                        bk = f"pb{bank}"
                        Hh.mm(self.pb[bank][0:nr, 0:64], gl[:, n0:n0 + nr], w2["v"][:, 0, :], True, True, ["wv2", "gl"], [bk])
                        Hh.copy("act", oc[0:nr, 0:64], self.pb[bank][0:nr, 0:64], [bk, "oc"], ["oc"])
                        Hh.dma(d["VCMP"][g, n0:n0 + nr, :], oc[0:nr, 0:64], ["oc"], [])
        P.flush()


K.phase_cmp = phase_cmp


def phase_imp(self, layer):
    nc, P, Hh, S = self.nc, self.P, self.Hh, self.S
    d = self.cl[layer // 2]
    NT = self.NT
    NCMP = S // 16 - 1
    NSEL = S // 64
    NKEEP = min(16, NSEL)
    with ExitStack() as es:
        sb = lambda name, shape, dt: self.sbuf(es, name, shape, dt)
        self.stg = [sb("stg0", [128, 2048], F32), sb("stg1", [128, 2048], F32)]
        self.stgi = 0
        F = self.load_const_bf16(es, "Fm", self.c_F, [128, 503])
        negi = self.load_const_bf16(es, "negi", self.c_negi, [128, 128])
        Eb = self.load_const_bf16(es, "Eb", self.c_E, [64, S])
        Hh.dma(d["EBF"], Eb[:], ["Eb"], [])
        PA = sb("PA", [128, 127], F32)
        PB = sb("PB", [128, 127], F32)
        Hh.dma(PA[:], self.c_PA, [], ["PA"])
        Hh.dma(PB[:], self.c_PB, [], ["PB"])
        kc = sb("kc", [64, 2, 256], BF16)
        Hh.dma(kc[:, :, 0:NCMP], d["KCMP"][:, :, 0:NCMP].rearrange("g d n -> d g n"), [], ["kc"])
        qt = [sb(f"qt{i}", [64, 16, 128], BF16) for i in range(2)]
        Psum = [sb(f"Ps{i}", [128, 264], F32) for i in range(2)]
        ex8 = [sb(f"ex8{i}", [128, 8, 256], F32) for i in range(2)]
        sm8 = [sb(f"sm8{i}", [128, 16], F32) for i in range(2)]
        imp = sb("imp", [128, 64], F32)
        imp2 = sb("imp2", [128, 64], F32)
        m8 = sb("m8", [128, 16], F32)
        thr = sb("thr", [128, 1], F32)
        selb = sb("selb", [128, 64], BF16)
        sbt = sb("sbt", [64, 128], BF16)
        pbn = rr([0, 1, 2, 3, 4, 5, 6, 7])
        ei = 0
        for i in range(NT):
            p = i % 2
            if i == 0:
                Hh.dma(qt[0][:], d["QC"][:, :, 0:128].rearrange("h d t -> d h t"), [], ["qt0"])
            if i + 1 < NT:
                Hh.dma(qt[(i + 1) % 2][:], d["QC"][:, :, (i + 1) * 128:(i + 2) * 128].rearrange("h d t -> d h t"), [], [f"qt{(i + 1) % 2}"])
            N = min(NCMP, 8 * i + 7)
            for g in range(2):
                Pk = f"Ps{g}"
                Hh.memset("pool", Psum[g][:], 0.0, [Pk])
                for hh in range(8):
                    h = g * 8 + hh
                    bank = pbn()
                    bk = f"pb{bank}"
                    Hh.mm(self.pb[bank][:, 0:N], qt[p][:, h, :], kc[:, g, 0:N], True, False, [f"qt{p}", "kc"], [bk])
                    Hh.mm(self.pb[bank][:, 0:N], self.ident[:], F[:, 248 - 8 * i:248 - 8 * i + N], False, True, ["ident", "Fm"], [bk])
                    Hh.act(ex8[g][:, hh, 0:N], self.pb[bank][:, 0:N], AF.Exp, [bk, f"ex8{g}"], [f"ex8{g}"], accum_out=sm8[g][:, hh:hh + 1])
                Hh.ts("dve", sm8[g][:, 8:16], sm8[g][:, 0:8], 1e-30, None, ALU.max, None, [f"ex8{g}"], [f"sm8{g}"])
                Hh.recip(sm8[g][:, 8:16], sm8[g][:, 8:16], [f"sm8{g}"], [f"sm8{g}"])
                for hh in range(8):
                    Hh.stt("dve", Psum[g][:, 1:1 + N], ex8[g][:, hh, 0:N], sm8[g][:, 8 + hh:9 + hh], Psum[g][:, 1:1 + N], ALU.mult, ALU.add,
                           [f"ex8{g}", f"sm8{g}", Pk], [Pk])
                Bv = Psum[g][:, 0:256].rearrange("p (j f) -> p j f", f=4)
                Hh.tt("dve", imp[:, 0:NSEL], Bv[:, 0:NSEL, 0], Bv[:, 0:NSEL, 1], ALU.add, [Pk, "imp"], ["imp"])
                Hh.tt("dve", imp[:, 0:NSEL], imp[:, 0:NSEL], Bv[:, 0:NSEL, 2], ALU.add, [Pk, "imp"], ["imp"])
                Hh.tt("dve", imp[:, 0:NSEL], imp[:, 0:NSEL], Bv[:, 0:NSEL, 3], ALU.add, [Pk, "imp"], ["imp"])
                B4 = Psum[g][:, 4:260].rearrange("p (j f) -> p j f", f=4)
                Hh.tt("dve", imp[:, 0:NSEL], imp[:, 0:NSEL], B4[:, 0:NSEL, 0], ALU.add, [Pk, "imp"], ["imp"])
                o0 = 63 - 2 * i
                Hh.tt("dve", imp[:, 0:NSEL], imp[:, 0:NSEL], PA[:, o0:o0 + NSEL], ALU.mult, ["imp", "PA"], ["imp"])
                Hh.tt("dve", imp[:, 0:NSEL], imp[:, 0:NSEL], PB[:, o0:o0 + NSEL], ALU.add, ["imp", "PB"], ["imp"])
                if i > 0:
                    Hh.memset("dve", imp[:, 0:1], 30000.0, ["imp"])
                if NSEL < 64:
                    Hh.memset("dve", imp[:, NSEL:64], -1e30, ["imp"])
                P.op("dve", lambda e_: e_.max(out=m8[:, 0:8], in_=imp[:]), ["imp", "m8"], ["m8"])
                P.op("dve", lambda e_: e_.match_replace(out=imp2[:], in_to_replace=m8[:, 0:8], in_values=imp[:], imm_value=-1e30),
                     ["imp", "m8", "imp2"], ["imp2"])
                P.op("dve", lambda e_: e_.max(out=m8[:, 8:16], in_=imp2[:]), ["imp2", "m8"], ["m8b"])
                if NKEEP >= 16:
                    P.op("dve", lambda e_: e_.tensor_reduce(out=thr[:], in_=m8[:, 8:16], axis=AX.X, op=ALU.min), ["m8b", "thr"], ["thr"])
                else:
                    P.op("dve", lambda e_: e_.tensor_reduce(out=thr[:], in_=m8[:, 0:NKEEP], axis=AX.X, op=ALU.min), ["m8", "m8b", "thr"], ["thr"])
                Hh.ts("dve", thr[:], thr[:], -1e29, None, ALU.max, None, ["thr"], ["thr"])
                Hh.ts("dve", selb[:], imp[:], thr[:, 0:1], None, ALU.is_lt, None, ["imp", "thr", "selb"], ["selb"])
                bank = pbn()
                bk = f"pb{bank}"
                Hh.mm(self.pb[bank][0:64, 0:128], selb[:], negi[:], True, True, ["selb", "negi"], [bk])
                Hh.copy("act", sbt[:], self.pb[bank][0:64, 0:128], [bk, "sbt"], ["sbt"])
                Hh.dma(d["SBT"][g, :, i * 128:(i + 1) * 128], sbt[:], ["sbt"], [])
        P.flush()


K.phase_imp = phase_imp


def phase_attn_c(self, layer):
    nc, P, Hh, S = self.nc, self.P, self.Hh, self.S
    d = self.cl[layer // 2]
    NT = self.NT
    NCMP = S // 16 - 1
    for br in self.cfg.get("c_br", [0, 1, 2]):
        with ExitStack() as es:
            sb = lambda name, shape, dt: self.sbuf(es, name, shape, dt)
            self.stg = [sb("stg0", [128, 2048], F32), sb("stg1", [128, 2048], F32)]
            self.stgi = 0
            G = sb("G", [128, NT, 48], F32)
            Hh.dma(G[:], d["G"], [], ["gate"])
            cm = self.load_const_bf16(es, "cm", self.c_cm, [128, 4, 512])
            heads = []
            if br == 0:
                cmpm = self.load_const_bf16(es, "cmpm", self.c_cmpm, [128, 5, 512])

                def kts0(j):
                    r = [0]
                    if NCMP > 128 and 16 * 128 + 31 <= 512 * j + 511:
                        r.append(1)
                    return r

                def terms0(j, kt, par):
                    m = j - 4 * kt
                    if m >= 5:
                        return []
                    kr = min(128, NCMP - kt * 128)
                    return [(self.ident[0:kr, 0:kr], cmpm[0:kr, m, :], ("ident", "cmpm"))]
                for h in range(16):
                    g = h // 8
                    heads.append(dict(q=d["QC"][h], k=d["KCMP"][g][:, 0:NCMP], Kd=64, v=d["VCMP"][g], ocol=h * 64,
                                      gate=(lambda j, c, h=h: G[:, 4 * j + c, h * 3:h * 3 + 1]), kts=kts0, terms=terms0))
                self.attention(es, heads, 512, d["OC"], NCMP, tagp="cmp")
            elif br == 1:

                def mk_terms(g):
                    def terms1(j, kt, par):
                        r = []
                        if kt >= 4 * j:
                            r.append((self.ident[:], cm[:, kt - 4 * j, :], ("ident", "cm")))
                        return r
                    return terms1
                for h in range(16):
                    g = h // 8
                    heads.append(dict(q=d["QC"][h], k=d["KS"][g], Kd=128, Kbase=64, q_extra=d["SBT"][g], k_extra=d["EBF"],
                                      v=d["VS"][:, g * 64:(g + 1) * 64], ocol=h * 64,
                                      gate=(lambda j, c, h=h: G[:, 4 * j + c, h * 3 + 1:h * 3 + 2]),
                                      kts=(lambda j: list(range(4 * j + 4))), terms=mk_terms(g)))
                self.attention(es, heads, 512, d["OS"], S, tagp="sel")
            else:
                wm = self.load_const_bf16(es, "wm", self.c_wm, [128, 4, 512])

                def terms2(j, kt, par):
                    if kt >= 4 * j:
                        return [(self.ident[:], cm[:, kt - 4 * j, :], ("ident", "cm"))]
                    return [(self.ident[:], wm[:, kt - (4 * j - 4), :], ("ident", "wm"))]
                for h in range(16):
                    g = h // 8
                    heads.append(dict(q=d["QC"][h], k=d["KW"][g], Kd=64, v=d["VW"][:, g * 64:(g + 1) * 64], ocol=h * 64,
                                      gate=(lambda j, c, h=h: G[:, 4 * j + c, h * 3 + 2:h * 3 + 3]),
                                      kts=(lambda j: list(range(max(0, 4 * j - 4), 4 * j + 4))), terms=terms2))
                self.attention(es, heads, 512, d["OW"], S, tagp="win")
            P.flush()


K.phase_attn_c = phase_attn_c


def layer_c(self, layer, h_in, h_out):
    d = self.cl[layer // 2]
    stop = self.cfg.get("c_stop", 99)
    self.phase_proj_c(layer, h_in)
    if stop <= 1:
        return
    self.phase_cmp(layer)
    if stop <= 2:
        return
    self.phase_imp(layer)
    if stop <= 3:
        return
    self.phase_attn_c(layer)
    self.phase_out([d["OC"], d["OS"], d["OW"]], d["w_out"], h_in, h_out)


K.layer_c = layer_c


SEQ = 4096
NCORES = 8


def kernel(**inputs):
    inp = {k: np.asarray(v) for k, v in inputs.items()}
    cfg = dict(layers=[0, 1, 2, 3], mixer=True)
    kb_ = K(SEQ, cfg)
    nc = kb_.build()
    in_maps = [host_inputs(SEQ, cfg, inp, b) for b in range(NCORES)]
    res = run_bass_kernel_spmd(nc, in_maps, core_ids=list(range(NCORES)))
    return np.stack([np.asarray(r["out"], dtype=np.float32) for r in res.results], 0)
```

```python
from concourse.bass_utils import run_bass_kernel_spmd
import numpy as np
import concourse.bass as bass
import concourse.mybir as mybir

F32 = mybir.dt.float32
BF16 = mybir.dt.bfloat16
ALU = mybir.AluOpType
AF = mybir.ActivationFunctionType
AX = mybir.AxisListType

ENGINES = ("pe", "act", "dve", "pool", "sp")
EPOCH = 16000
NEPOCH = {"pe": 10, "act": 8, "dve": 10, "pool": 6, "sp": 1}
NDMA_SEM = 8


class Prog:
    def __init__(self, nc, es):
        self.nc = nc
        self.ops = []
        self.esem = {e: [es.enter_context(nc.semaphore(f"s_{e}_{k}")) for k in range(NEPOCH[e])]
                     for e in ENGINES}
        self.dsem = {e: [es.enter_context(nc.semaphore(f"d_{e}_{k}")) for k in range(NDMA_SEM)]
                     for e in ("sp", "act", "pool")}
        self.bsem = es.enter_context(nc.semaphore("bar"))
        self.cnt = {e: 0 for e in ENGINES}
        self.dcnt = {e: 0 for e in ENGINES}
        self.seen = {e: {} for e in ENGINES}
        self.nphase = 0
        self.nops_total = 0

    def op(self, eng, fn, reads=(), writes=(), dma=False):
        pbr = [r for r in reads if isinstance(r, str) and r.startswith("pb")]
        if pbr:
            reads = [r for r in reads if r not in pbr]
            writes = list(writes) + [r for r in pbr if r not in writes]
        self.ops.append(dict(eng=eng, fn=fn, reads=tuple(reads), writes=tuple(writes), dma=dma))

    def dma(self, out, in_, reads=(), writes=(), eng="sp", **kw):
        self.op(eng, lambda e: e.dma_start(out=out, in_=in_, **kw), reads, writes, dma=True)

    def analyze(self):
        ops = self.ops
        last_w = {}
        readers = {}
        for i, o in enumerate(ops):
            deps = set()
            for r in o["reads"]:
                if r in last_w:
                    deps.add(last_w[r])
            for w in o["writes"]:
                if w in last_w:
                    deps.add(last_w[w])
                for j in readers.get(w, ()):
                    deps.add(j)
            deps.discard(i)
            fd = []
            for j in deps:
                oj = ops[j]
                if (not o["dma"]) and (not oj["dma"]) and o["eng"] == "pe" and oj["eng"] == "pe":
                    continue
                fd.append(j)
            o["deps"] = fd
            for j in fd:
                ops[j]["needs_inc"] = True
            for r in o["reads"]:
                readers.setdefault(r, []).append(i)
            for w in o["writes"]:
                last_w[w] = i
                readers[w] = []
        lastop = {}
        for i, o in enumerate(ops):
            if not o["dma"]:
                lastop[o["eng"]] = i
        for e, i in lastop.items():
            ops[i]["needs_inc"] = True
        for o in ops:
            e = o["eng"]
            if o["dma"]:
                n = self.dcnt[e]
                self.dcnt[e] += 1
                o["dslot"] = n % NDMA_SEM
                o["dtarget"] = 16 * (n // NDMA_SEM + 1)
            elif o.get("needs_inc"):
                n = self.cnt[e]
                self.cnt[e] += 1
                o["epoch"] = n // EPOCH
                o["ord"] = n % EPOCH + 1
                assert o["epoch"] < NEPOCH[e], f"too many incs on {e}"

    def flush(self):
        nc = self.nc
        self.analyze()
        ops = self.ops
        self.nops_total += len(ops)
        self.nphase += 1
        phase = self.nphase
        esem, dsem, bsem = self.esem, self.dsem, self.bsem

        def run_engine(e, eng):
            seen = self.seen[e]

            def wait(sem, key, val):
                if seen.get(key, 0) >= val:
                    return
                eng.wait_ge(sem, val)
                seen[key] = val

            for o in ops:
                if o["eng"] != e:
                    continue
                if o["dma"]:
                    prev = o["dtarget"] - 16
                    if prev > 0:
                        wait(dsem[e][o["dslot"]], ("d", e, o["dslot"]), prev)
                for j in o["deps"]:
                    oj = ops[j]
                    if oj["dma"]:
                        wait(dsem[oj["eng"]][oj["dslot"]], ("d", oj["eng"], oj["dslot"]), oj["dtarget"])
                    else:
                        wait(esem[oj["eng"]][oj["epoch"]], ("e", oj["eng"], oj["epoch"]), oj["ord"])
                ins = o["fn"](eng)
                if o["dma"]:
                    ins.then_inc(dsem[e][o["dslot"]], 16)
                elif o.get("needs_inc"):
                    ins.then_inc(esem[e][o["epoch"]], 1)
            n = self.cnt[e]
            if n > 0:
                ep, od = (n - 1) // EPOCH, (n - 1) % EPOCH + 1
                wait(esem[e][ep], ("e", e, ep), od)
            if e in dsem:
                nd = self.dcnt[e]
                for k in range(NDMA_SEM):
                    tot = (nd - k + NDMA_SEM - 1) // NDMA_SEM if nd > k else 0
                    if tot > 0:
                        wait(dsem[e][k], ("d", e, k), 16 * tot)
            eng.sem_inc(bsem, 1)
            eng.wait_ge(bsem, 5 * phase)

        with nc.Block() as block:
            @block.sync
            def _(eng):
                run_engine("sp", eng)

            @block.tensor
            def _(eng):
                run_engine("pe", eng)

            @block.scalar
            def _(eng):
                run_engine("act", eng)

            @block.vector
            def _(eng):
                run_engine("dve", eng)

            @block.gpsimd
            def _(eng):
                run_engine("pool", eng)
        self.ops = []


import numpy as np
from contextlib import ExitStack

D = 1024
DFF = 4096
NEG = -30000.0
STORE_ENG = "sp"


class H:
    def __init__(self, P):
        self.P = P

    def mm(self, out, lhsT, rhs, start, stop, reads, writes, skip=False):
        kw = dict(skip_group_check=True) if skip else {}
        self.P.op("pe", lambda e: e.matmul(out, lhsT=lhsT, rhs=rhs, start=start, stop=stop, **kw), reads, writes)

    def act(self, out, in_, func, reads, writes, bias=None, scale=None, accum_out=None):
        kw = {}
        if bias is not None:
            kw["bias"] = bias
        if scale is not None:
            kw["scale"] = scale
        if accum_out is not None:
            kw["accum_out"] = accum_out
        self.P.op("act", lambda e: e.activation(out=out, in_=in_, func=func, **kw), reads, writes)

    def ts(self, eng, out, in0, s1, s2, op0, op1, reads, writes, accum_out=None):
        kw = {}
        if op1 is not None:
            kw["op1"] = op1
        if accum_out is not None:
            kw["accum_out"] = accum_out
        self.P.op(eng, lambda e: e.tensor_scalar(out=out, in0=in0, scalar1=s1, scalar2=s2, op0=op0, **kw), reads, writes)

    def tt(self, eng, out, in0, in1, op, reads, writes):
        self.P.op(eng, lambda e: e.tensor_tensor(out=out, in0=in0, in1=in1, op=op), reads, writes)

    def stt(self, eng, out, in0, scalar, in1, op0, op1, reads, writes):
        self.P.op(eng, lambda e: e.scalar_tensor_tensor(out=out, in0=in0, scalar=scalar, in1=in1, op0=op0, op1=op1),
                  reads, writes)

    def copy(self, eng, out, in_, reads, writes):
        if eng == "act":
            self.P.op("act", lambda e: e.copy(out=out, in_=in_), reads, writes)
        else:
            self.P.op(eng, lambda e: e.tensor_copy(out=out, in_=in_), reads, writes)

    def memset(self, eng, ap, val, writes):
        self.P.op(eng, lambda e: e.memset(ap, val), [], writes)

    def recip(self, out, in_, reads, writes):
        self.P.op("dve", lambda e: e.reciprocal(out=out, in_=in_), reads, writes)

    def dma(self, out, in_, reads, writes, eng=None):
        if eng is None:
            eng = STORE_ENG if type(out.tensor).__name__ == "DRamTensorHandle" else "sp"
        self.P.dma(out, in_, reads=reads, writes=writes, eng=eng)


class Ctx:
    pass


def rr(lst):
    i = [0]

    def nxt():
        v = lst[i[0] % len(lst)]
        i[0] += 1
        return v
    return nxt


import numpy as np
from contextlib import ExitStack

EPS = 1e-6


class K:
    def __init__(self, S, cfg):
        self.S = S
        self.cfg = cfg
        self.nc = bass.Bass("TRN2", target_bir_lowering=False)
        self.inputs = {}
        self.NT = S // 128

    def din(self, name, shape, dt=F32):
        t = self.nc.dram_tensor(name, list(shape), dt, kind="ExternalInput").ap()
        self.inputs[name] = (tuple(shape), dt)
        return t

    def sbuf(self, es, name, shape, dt):
        self.uid = getattr(self, "uid", 0) + 1
        return es.enter_context(self.nc.sbuf_tensor(f"{name}_u{self.uid}", shape, dt))

    def dscr(self, name, shape, dt):
        if self.cfg.get("dbg") and name in self.cfg["dbg"]:
            return self.nc.dram_tensor(name, list(shape), dt, kind="ExternalOutput").ap()
        return self.nc.dram_tensor(name, list(shape), dt).ap()

    def consts(self, es):
        nc, P, Hh = self.nc, self.P, self.Hh
        sb = lambda name, shape, dt: self.sbuf(es, name, shape, dt)
        self.ident = sb("ident", [128, 128], BF16)
        self.identf = sb("identf", [128, 128], F32)
        self.epsc = sb("epsc", [128, 1], F32)
        self.pb = [es.enter_context(nc.psum_tensor(f"pb{i}", [128, 512], F32)) for i in range(8)]
        d_ident = self.din("c_ident", [128, 128])
        Hh.dma(self.identf[:], d_ident, [], ["identf"])
        Hh.copy("dve", self.ident[:], self.identf[:], ["identf"], ["ident"])
        Hh.memset("dve", self.epsc[:], EPS, ["epsc"])
        P.flush()

    def rmsnorm_T(self, hs, sub, uT, ntok_off, gain_unused, tag, pbn, scr):
        Hh = self.Hh
        ss, rstd, ub, junk = scr["ss"], scr["rstd"], scr["ub"], scr["junk"]
        hk = scr["hkey"]
        Hh.act(junk[:], hs, AF.Square, [hk, "junk" + tag], ["junk" + tag, "ss" + tag], accum_out=ss[:, 0:1])
        Hh.act(rstd[:, 0:1], ss[:, 0:1], AF.Sqrt, ["ss" + tag, "epsc"], ["rstd" + tag], bias=self.epsc[:, 0:1], scale=1.0 / D)
        Hh.recip(rstd[:, 1:2], rstd[:, 0:1], ["rstd" + tag], ["rstd2" + tag])
        Hh.act(ub[:], hs, AF.Copy, [hk, "rstd2" + tag, "ub" + tag], ["ub" + tag], scale=rstd[:, 1:2])
        for half in range(2):
            bank = pbn()
            bk = f"pb{bank}"
            for q in range(4):
                k = half * 4 + q
                Hh.mm(self.pb[bank][:, q * 128:(q + 1) * 128], ub[:, k * 128:(k + 1) * 128], self.ident[:],
                      True, True, ["ub" + tag, "ident"], [bk])
            Hh.copy("dve", uT[:, half * 4:half * 4 + 4, ntok_off:ntok_off + 128],
                    self.pb[bank][:].rearrange("p (q t) -> p q t", q=4), [bk], [scr["uTkey"]])

    def load_weight_bf16(self, es_w, name, dram, kchunks, ncols, gain=None, colsplit=2048, tagp="", colkeys=False):
        nc, Hh = self.nc, self.Hh
        w = self.sbuf(es_w, name, [128, kchunks, ncols], BF16)
        stg = self.stg
        i = 0
        order = [(k, c0) for k in range(kchunks) for c0 in range(0, ncols, colsplit)]
        if colkeys:
            order.sort(key=lambda kc: (kc[1], kc[0]))
        for (k, c0) in order:
            if True:
                cw = min(colsplit, ncols - c0)
                s = stg[self.stgi % 2]
                sk = f"stg{self.stgi % 2}"
                self.stgi += 1
                Hh.dma(s[:, 0:cw], dram[k * 128:(k + 1) * 128, c0:c0 + cw], [], [sk])
                eng = "pool" if (i % 2 == 0) else "dve"
                i += 1
                wkey = name + (f"_c{c0 // colsplit}" if colkeys else "")
                if gain is not None:
                    Hh.ts(eng, w[:, k, c0:c0 + cw], s[:, 0:cw], gain[:, k:k + 1], None, ALU.mult, None,
                          [sk, "gain" + tagp], [wkey])
                else:
                    Hh.copy(eng, w[:, k, c0:c0 + cw], s[:, 0:cw], [sk], [wkey])
        return w

    def phase_mlp(self, layer, h_in, h_out):
        nc, P, Hh, S = self.nc, self.P, self.Hh, self.S
        w1d = self.w_mlp1[layer]
        w2d = self.w_mlp2[layer]
        gd = self.g_mlp[layer]
        TT = 256
        with ExitStack() as es:
            sb = lambda name, shape, dt: self.sbuf(es, name, shape, dt)
            self.stg = [sb("stg0", [128, 2048], F32), sb("stg1", [128, 2048], F32)]
            self.stgi = 0
            gain = sb("gain", [128, 8], F32)
            Hh.dma(gain[:], gd, [], ["gain"])
            w1 = self.load_weight_bf16(es, "w1", w1d, 8, DFF, gain=gain, colkeys=True)
            w2 = self.load_weight_bf16(es, "w2", w2d, 32, D)
            hs = [sb(f"hs{i}", [128, D], F32) for i in range(4)]
            uT = [sb(f"uT{i}", [128, 8, TT], BF16) for i in range(2)]
            h1T = sb("h1T", [128, 32, TT], BF16)
            rl = [sb(f"rl{i}", [128, 512], F32) for i in range(2)]
            scr = [dict(ss=sb(f"ss{i}", [128, 1], F32), rstd=sb(f"rstd{i}", [128, 2], F32),
                        ub=sb(f"ub{i}", [128, D], BF16), junk=sb(f"junk{i}", [128, D], F32)) for i in range(2)]
            pbn = rr([0, 1, 2, 3])
            pacc = [4, 5, 6, 7]
            ntt = S // TT
            def mlp_loads(tt_):
                for sub_ in range(2):
                    hi_ = (tt_ % 2) * 2 + sub_
                    r0_ = tt_ * TT + sub_ * 128
                    Hh.dma(hs[hi_][:], h_in[r0_:r0_ + 128, :], [], [f"hs{hi_}"])
            mlp_loads(0)
            for tt in range(ntt):
                u = uT[tt % 2]
                uk = f"uT{tt % 2}"
                if tt + 1 < ntt:
                    mlp_loads(tt + 1)
                for sub in range(2):
                    hi = (tt % 2) * 2 + sub
                    hk = f"hs{hi}"
                    r0 = tt * TT + sub * 128
                    sc = scr[sub]
                    sc["hkey"] = hk
                    sc["uTkey"] = uk
                    self.rmsnorm_T(hs[hi][:], sub, u, sub * 128, None, f"m{sub}", pbn, sc)
                for fp in range(16):
                    bank = pbn()
                    bk = f"pb{bank}"
                    for q in range(2):
                        f = fp * 2 + q
                        for k in range(8):
                            Hh.mm(self.pb[bank][:, q * TT:(q + 1) * TT], w1[:, k, f * 128:(f + 1) * 128], u[:, k, :],
                                  k == 0, k == 7, [f"w1_c{(f * 128) // 2048}", uk], [bk])
                    r = rl[fp % 2]
                    rk = f"rl{fp % 2}"
                    Hh.act(r[:], self.pb[bank][:], AF.Relu, [bk, rk], [rk])
                    Hh.tt("pool", h1T[:, fp * 2:fp * 2 + 2, :], r[:].rearrange("p (q t) -> p q t", q=2),
                          r[:].rearrange("p (q t) -> p q t", q=2), ALU.mult, [rk, "h1T"], ["h1T"])
                for sub in range(2):
                    hi = (tt % 2) * 2 + sub
                    hk = f"hs{hi}"
                    for half in range(2):
                        bank = pacc[sub * 2 + half]
                        bk = f"pb{bank}"
                        for f in range(32):
                            Hh.mm(self.pb[bank][:], h1T[:, f, sub * 128:(sub + 1) * 128], w2[:, f, half * 512:(half + 1) * 512],
                                  f == 0, f == 31, ["h1T", "w2"], [bk])
                        Hh.tt("dve", hs[hi][:, half * 512:(half + 1) * 512], hs[hi][:, half * 512:(half + 1) * 512],
                              self.pb[bank][:], ALU.add, [bk, hk], [hk])
                    r0 = tt * TT + sub * 128
                    Hh.dma(h_out[r0:r0 + 128, :], hs[hi][:], [hk], [])
            P.flush()

    def phase_final(self, h_in, out):
        nc, P, Hh, S = self.nc, self.P, self.Hh, self.S
        with ExitStack() as es:
            sb = lambda name, shape, dt: self.sbuf(es, name, shape, dt)
            gb = sb("gfin", [128, D], F32)
            Hh.dma(gb[:], self.g_final, [], ["gfin"])
            hs = [sb(f"hs{i}", [128, D], F32) for i in range(2)]
            junk = [sb(f"junk{i}", [128, D], F32) for i in range(2)]
            ss = [sb(f"ss{i}", [128, 1], F32) for i in range(2)]
            rstd = [sb(f"rstd{i}", [128, 2], F32) for i in range(2)]
            Hh.dma(hs[0][:], h_in[0:128, :], [], ["hs0"])
            for t in range(self.NT):
                i = t % 2
                hk, jk, sk, rk = f"hs{i}", f"junk{i}", f"ss{i}", f"rstd{i}"
                if t + 1 < self.NT:
                    Hh.dma(hs[(t + 1) % 2][:], h_in[(t + 1) * 128:(t + 2) * 128, :], [], [f"hs{(t + 1) % 2}"])
                Hh.act(junk[i][:], hs[i][:], AF.Square, [hk, jk], [jk, sk], accum_out=ss[i][:, 0:1])
                Hh.act(rstd[i][:, 0:1], ss[i][:, 0:1], AF.Sqrt, [sk, "epsc"], [rk], bias=self.epsc[:, 0:1], scale=1.0 / D)
                Hh.recip(rstd[i][:, 1:2], rstd[i][:, 0:1], [rk], [rk + "b"])
                Hh.stt("dve", junk[i][:], hs[i][:], rstd[i][:, 1:2], gb[:], ALU.mult, ALU.mult, [hk, rk + "b", "gfin", jk], [jk])
                Hh.dma(out[t * 128:(t + 1) * 128, :], junk[i][:], [jk], [])
            P.flush()

    def build(self):
        nc, S, cfg = self.nc, self.S, self.cfg
        layers = cfg["layers"]
        NT = self.NT
        self.x = self.din("x", [S, D])
        self.out = nc.dram_tensor("out", [S, D], F32, kind="ExternalOutput").ap()
        self.w_mlp1, self.w_mlp2, self.g_mlp = {}, {}, {}
        for l in layers:
            self.w_mlp1[l] = self.din(f"mlp_w1_{l}", [D, DFF])
            self.w_mlp2[l] = self.din(f"mlp_w2_{l}", [DFF, D])
            self.g_mlp[l] = self.din(f"mlp_g_{l}", [128, 8])
        self.g_final = self.din("g_final", [128, D])
        self.hA = self.dscr("hA", [S, D], F32)
        self.hB = self.dscr("hB", [S, D], F32)
        self.ab, self.cl = {}, {}
        if cfg.get("mixer", True):
            self.ropetab = self.din("c_ropetab", [4, 128, S])
            self.c_caus = self.din("c_caus", [128, 128])
            self.c_pow2 = self.din("c_pow2", [128, 24])
            self.c_cm = self.din("c_cm", [128, 4, 512])
            self.c_negi = self.din("c_negi", [128, 128])
            self.c_perm = self.din("c_perm", [128, 128])
            for l in layers:
                j = l // 2
                if l % 2 == 0:
                    d = {}
                    d["wab"] = self.din(f"ab_w_{j}", [D, AB_NC])
                    d["g_attn"] = self.din(f"ab_g_{j}", [128, 8])
                    d["wuk"] = self.din(f"ab_wuk_{j}", [256, 512])
                    d["wuv"] = self.din(f"ab_wuv_{j}", [256, 512])
                    d["g_kv"] = self.din(f"ab_gkv_{j}", [128, 2])
                    d["g_ki"] = self.din(f"ab_gki_{j}", [64, 2])
                    d["b_forget"] = self.din(f"ab_bf_{j}", [8, 1])
                    d["w_out"] = self.din(f"ab_wout_{j}", [D, D])
                    for nm, shp, dt in (("QA", [8, 64, S], BF16), ("KA", [8, 64, S], BF16), ("VA", [S, 512], BF16),
                                        ("QI", [8, 64, S], BF16), ("KI", [64, S], BF16), ("WI", [128, NT, 8], F32),
                                        ("BQ", [8, 66, S], BF16), ("BK", [8, 66, S], BF16), ("VB", [S, 512], BF16),
                                        ("BFT", [8, S], F32), ("NCUM", [128, NT, 8], F32), ("MB", [S, S], BF16),
                                        ("OCAT", [S, D], BF16)):
                        d[nm] = self.dscr(f"{nm}_{j}", shp, dt)
                    self.ab[j] = d
                else:
                    self.decl_c(j)
        with ExitStack() as es:
            self.P = Prog(nc, es)
            self.Hh = H(self.P)
            self.consts(es)
            h = self.x
            for l in layers:
                if cfg.get("mixer", True):
                    if l % 2 == 0:
                        self.layer_ab(l, h, self.hA)
                    else:
                        self.layer_c(l, h, self.hA)
                    h = self.hA
                self.phase_mlp(l, h, self.hB)
                h = self.hB
            self.phase_final(h, self.out)
        return nc


def host_inputs(S, cfg, inp, b):
    m = {}
    m["x"] = np.ascontiguousarray(inp["x"][b, :S])
    m["c_ident"] = np.eye(128, dtype=np.float32)
    for l in cfg["layers"]:
        m[f"mlp_w1_{l}"] = np.ascontiguousarray(inp["mlp_w1"][l])
        m[f"mlp_w2_{l}"] = np.ascontiguousarray(inp["mlp_w2"][l])
        m[f"mlp_g_{l}"] = np.ascontiguousarray(inp["mlp_norm"][l].reshape(8, 128).T)
    m["g_final"] = np.ascontiguousarray(np.broadcast_to(inp["final_norm"][None, :], (128, D)))
    if cfg.get("mixer", True):
        cq, sq = rope_tables_host(S, 0.125)
        ck, sk = rope_tables_host(S, 1.0)
        m["c_ropetab"] = np.ascontiguousarray(np.stack([cq, sq, ck, sk], 0))
        pp = np.arange(128)[:, None]
        cc = np.arange(128)[None, :]
        m["c_caus"] = np.where(cc <= pp, 0.0, -1e30).astype(np.float32)
        m["c_pow2"] = np.ascontiguousarray(np.broadcast_to((2.0 ** -(np.arange(24) + 1.0))[None, :], (128, 24))).astype(np.float32)
        c5 = np.arange(512)[None, None, :]
        r4 = np.arange(4)[None, :, None]
        m["c_cm"] = np.where(128 * r4 + pp[:, :, None] <= c5, 0.0, NEG).astype(np.float32)
        m["c_negi"] = (NEG * np.eye(128)).astype(np.float32)
        pm = np.zeros((128, 128), np.float32)
        for hb in (0, 64):
            for dd in range(8):
                pm[hb + dd + 8, hb + dd] = 1.0
                pm[hb + dd, hb + dd + 8] = 1.0
        m["c_perm"] = pm
        for l in cfg["layers"]:
            j = l // 2
            if l % 2 == 0:
                m[f"ab_w_{j}"] = ab_weight_layout(inp["ab_w_in"][j])[0]
                m[f"ab_g_{j}"] = np.ascontiguousarray(inp["attn_norm"][l].reshape(8, 128).T)
                wuk = np.zeros((256, 8, 64), np.float32)
                wuk[:, :, 16:] = inp["a_w_uk"][j]
                m[f"ab_wuk_{j}"] = wuk.reshape(256, 512)
                m[f"ab_wuv_{j}"] = np.ascontiguousarray(inp["a_w_uv"][j].reshape(256, 512))
                m[f"ab_gkv_{j}"] = np.ascontiguousarray(inp["a_kv_norm"][j].reshape(2, 128).T)
                g = inp["a_kidx_norm"][j]
                grot = np.zeros(64, np.float32)
                grot[0:8] = g[8:16]
                grot[8:16] = g[0:8]
                m[f"ab_gki_{j}"] = np.ascontiguousarray(np.stack([g, grot], 1))
                m[f"ab_bf_{j}"] = np.ascontiguousarray(inp["ab_b_forget"][j].reshape(8, 1))
                m[f"ab_wout_{j}"] = np.ascontiguousarray(inp["ab_w_out"][j])
            else:
                host_inputs_c(m, S, inp, l)
    return m


def attention(self, es, heads, QW, out_dram, NK, unit_load=None, tagp="at"):
    nc, P, Hh, S = self.nc, self.P, self.Hh, self.S
    sb = lambda name, shape, dt: self.sbuf(es, name, shape, dt)
    NKT = (NK + 127) // 128
    C = QW // 128
    NJ = S // QW
    qs = [sb(f"qs{i}", [128, S], BF16) for i in range(2)]
    ks = [sb(f"ks{i}", [128, NKT * 128], BF16) for i in range(2)]
    vs = [sb(f"vs{i}", [128, NKT, 65], BF16) for i in range(2)]
    PT = [sb(f"pt{i}", [128, QW], BF16) for i in range(3)]
    ot = [sb(f"ot{i}", [128, C, 64], BF16) for i in range(2)]
    rc = [sb(f"rc{i}", [128, 2 * C], F32) for i in range(2)]
    for i in range(2):
        Hh.memset("pool", vs[i][:, :, 64:65], 1.0, [f"vs{i}"])

    def load_head(hi):
        hd = heads[hi]
        p = hi % 2
        Kd = hd["Kd"]
        Kb = hd.get("Kbase", Kd)
        Hh.dma(qs[p][0:Kb, :], hd["q"], [], [f"qs{p}"])
        Hh.dma(ks[p][0:Kb, 0:NK], hd["k"], [], [f"ks{p}"])
        if hd.get("q_extra") is not None:
            Hh.dma(qs[p][Kb:Kd, :], hd["q_extra"], [], [f"qs{p}"])
            Hh.dma(ks[p][Kb:Kd, 0:NK], hd["k_extra"], [], [f"ks{p}"])
        nfull = NK // 128
        if nfull > 0:
            Hh.dma(vs[p][:, 0:nfull, 0:64], hd["v"][0:nfull * 128, :].rearrange("(n p) c -> p n c", p=128), [], [f"vs{p}"])
        rem = NK - nfull * 128
        if rem > 0:
            Hh.dma(vs[p][0:rem, nfull, 0:64], hd["v"][nfull * 128:NK, :], [], [f"vs{p}"])

    work = []
    units = []
    for hi, hd in enumerate(heads):
        for j in range(NJ):
            kts = hd["kts"](j)
            if not kts:
                continue
            u = len(units)
            units.append((hi, j, kts))
            for n, kt in enumerate(kts):
                work.append((u, hi, j, kt, n == 0, n == len(kts) - 1))
    sbanks = rr([0, 1, 2, 3, 4, 5])
    wstate = {}

    def emit_qk(w):
        u, hi, j, kt, first, last = w
        hd = heads[hi]
        p = hi % 2
        Kd = hd["Kd"]
        kr = min(128, NK - kt * 128)
        bank = sbanks()
        bk = f"pb{bank}"
        terms = hd["terms"](j, kt, u % 3) if hd.get("terms") else []
        Hh.mm(self.pb[bank][0:kr, 0:QW], ks[p][0:Kd, kt * 128:kt * 128 + kr], qs[p][0:Kd, j * QW:(j + 1) * QW],
              True, len(terms) == 0, [f"ks{p}", f"qs{p}"], [bk])
        for ti, (l, r, keys) in enumerate(terms):
            Hh.mm(self.pb[bank][0:kr, 0:QW], l, r, False, ti == len(terms) - 1, list(keys), [bk])
        wstate[w] = bank

    def emit_rest(w, idx):
        u, hi, j, kt, first, last = w
        hd = heads[hi]
        p = hi % 2
        kr = min(128, NK - kt * 128)
        bank = wstate.pop(w)
        bk = f"pb{bank}"
        pi = idx % 3
        kb = hd["kbias"](kt) if hd.get("kbias") else None
        if kb is not None and kr < 128:
            kb = kb[0:kr, :]
        rd = [bk, f"pt{pi}"] + (["kbias"] if kb is not None else [])
        Hh.act(PT[pi][0:kr, 0:QW], self.pb[bank][0:kr, 0:QW], AF.Exp, rd, [f"pt{pi}"], bias=kb)
        ab = 6 + (u % 2)
        for c in range(C):
            Hh.mm(self.pb[ab][:, c * 65:(c + 1) * 65], PT[pi][0:kr, c * 128:(c + 1) * 128], vs[p][0:kr, kt, 0:65],
                  first and c == 0, last, [f"pt{pi}", f"vs{p}"], [f"pb{ab}"], skip=True)
        if last:
            oi = u % 2
            accv = self.pb[ab][:, 0:C * 65].rearrange("p (c e) -> p c e", e=65)
            Hh.ts("dve", rc[oi][:, 0:C], accv[:, :, 64], 1e-30, None, ALU.max, None, [f"pb{ab}"], [f"rc{oi}"])
            Hh.recip(rc[oi][:, C:2 * C], rc[oi][:, 0:C], [f"rc{oi}"], [f"rc{oi}"])
            for c in range(C):
                if hd.get("gate"):
                    Hh.tt("dve", rc[oi][:, C + c:C + c + 1], rc[oi][:, C + c:C + c + 1], hd["gate"](j, c), ALU.mult,
                          [f"rc{oi}", "gate"], [f"rc{oi}"])
                Hh.ts("dve", ot[oi][:, c, :], accv[:, c, 0:64], rc[oi][:, C + c:C + c + 1], None, ALU.mult, None,
                      [f"pb{ab}", f"rc{oi}", f"ot{oi}"], [f"ot{oi}"])
            oc = hd["ocol"]
            Hh.dma(out_dram[j * QW:(j + 1) * QW, oc:oc + 64].rearrange("(c p) d -> p c d", p=128), ot[oi][:], [f"ot{oi}"], [])

    LOOK = 2
    loaded = set()
    started_units = set()
    pending = []
    cnt = [0]

    def rest(w):
        emit_rest(w, cnt[0])
        cnt[0] += 1

    for w in work:
        u, hi, j, kt, first, last = w
        if hi not in loaded:
            while pending and pending[0][1] <= hi - 2:
                rest(pending.pop(0))
            load_head(hi)
            loaded.add(hi)
        if u not in started_units:
            started_units.add(u)
            if unit_load is not None:
                if u == 0:
                    unit_load(units[0][0], units[0][1], 0)
                if u + 1 < len(units):
                    unit_load(units[u + 1][0], units[u + 1][1], (u + 1) % 3)
        emit_qk(w)
        pending.append(w)
        while len(pending) > LOOK:
            rest(pending.pop(0))
        if hi + 1 < len(heads) and (hi + 1) not in loaded and all(pw[1] >= hi for pw in pending):
            load_head(hi + 1)
            loaded.add(hi + 1)
    while pending:
        rest(pending.pop(0))


K.attention = attention


AB_COLS = dict(aq=(0, 512), ackv=(512, 768), akr=(768, 784), aqi=(784, 1296), aki=(1296, 1360), awi=(1360, 1368),
               bq=(1368, 1880), bk=(1880, 2392), bv=(2392, 2904), bf=(2904, 2912))


def rot_cols(w, nheads, hd):
    w3 = w.reshape(w.shape[0], nheads, hd)
    o = np.zeros_like(w3)
    o[:, :, 0:8] = w3[:, :, 8:16]
    o[:, :, 8:16] = w3[:, :, 0:8]
    return o.reshape(w.shape)


def ab_weight_layout(w_in):
    g = lambda n: w_in[:, AB_COLS[n][0]:AB_COLS[n][1]]
    blocks = [("aq", g("aq")), ("aq_r", rot_cols(g("aq"), 8, 64)), ("aqi", g("aqi")), ("aqi_r", rot_cols(g("aqi"), 8, 64)),
              ("ackv", g("ackv")), ("akr", g("akr")), ("akr_r", rot_cols(g("akr"), 1, 16)),
              ("aki", g("aki")), ("aki_r", rot_cols(g("aki"), 1, 64)), ("awi", g("awi")),
              ("bq", g("bq")), ("bk", g("bk")), ("bv", g("bv")), ("bf", g("bf"))]
    offs, c = {}, 0
    for n, b in blocks:
        offs[n] = c
        c += b.shape[1]
    return np.ascontiguousarray(np.concatenate([b for _, b in blocks], axis=1)), offs, c


AB_OFFS = ab_weight_layout(np.zeros((1, 2912), np.float32))[1]
AB_NC = ab_weight_layout(np.zeros((1, 2912), np.float32))[2]


def rope_tables_host(S, scale):
    pos = np.arange(S, dtype=np.float32)
    inv = (500000.0 ** (-np.arange(0, 16, 2, dtype=np.float32) / 16)).astype(np.float32)
    ang = pos[None, :] * inv[:, None]
    c, s = np.cos(ang).astype(np.float32), np.sin(ang).astype(np.float32)
    C = np.ones((64, S), np.float32)
    Sn = np.zeros((64, S), np.float32)
    C[0:8] = c
    C[8:16] = c
    Sn[0:8] = -s
    Sn[8:16] = s
    C *= scale
    Sn *= scale
    return np.concatenate([C, C], 0), np.concatenate([Sn, Sn], 0)


def phase_proj_ab(self, layer, h_in):
    nc, P, Hh, S = self.nc, self.P, self.Hh, self.S
    j = layer // 2
    d = self.ab[j]
    O = AB_OFFS
    NQ = S // 512
    with ExitStack() as es:
        sb = lambda name, shape, dt: self.sbuf(es, name, shape, dt)
        self.stg = [sb("stg0", [128, 2048], F32), sb("stg1", [128, 2048], F32)]
        self.stgi = 0
        gain = sb("gain", [128, 8], F32)
        Hh.dma(gain[:], d["g_attn"], [], ["gain"])
        w = self.load_weight_bf16(es, "wab", d["wab"], 8, AB_NC, gain=gain)
        wuk = self.load_weight_bf16(es, "wuk", d["wuk"], 2, 512)
        wuv = self.load_weight_bf16(es, "wuv", d["wuv"], 2, 512)
        gkv = sb("gkv", [128, 2], F32)
        gki = sb("gki", [64, 2], F32)
        Hh.dma(gkv[:], d["g_kv"], [], ["gkv"])
        Hh.dma(gki[:], d["g_ki"], [], ["gki"])
        ones = sb("ones", [128, 128], BF16)
        Hh.memset("pool", ones[:], 1.0, ["ones"])
        hs = [sb(f"hs{i}", [128, D], F32) for i in range(4)]
        uT = [sb(f"uT{i}", [128, 8, 512], BF16) for i in range(2)]
        scr = [dict(ss=sb(f"ss{i}", [128, 1], F32), rstd=sb(f"rstd{i}", [128, 2], F32),
                    ub=sb(f"ub{i}", [128, D], BF16), junk=sb(f"junk{i}", [128, D], F32)) for i in range(2)]
        tabs = [sb(f"tab{i}", [128, 4, 512], F32) for i in range(2)]
        tmpA = [sb(f"tA{i}", [128, 512], F32) for i in range(3)]
        tmpB = [sb(f"tB{i}", [128, 512], F32) for i in range(3)]
        obf = [sb(f"ob{i}", [128, 512], BF16) for i in range(4)]
        xbf = [sb(f"xb{i}", [128, 512], BF16) for i in range(2)]
        perm = self.load_const_bf16(es, "perm", self.c_perm, [128, 128])
        sq = [sb(f"sq{i}", [128, 512], BF16) for i in range(2)]
        rbc = sb("rbc", [128, 512], F32)
        rbc2 = sb("rbc2", [128, 512], F32)
        ckvT = sb("ckvT", [128, 2, 512], BF16)
        akrT = sb("akrT", [16, 512], BF16)
        wis = sb("wis", [128, 4, 8], F32)
        pbn = rr([0, 1, 2, 3, 4, 5])
        cnt = dict(t=0, o=0, x=0)

        def fm(u, uk, c0, M):
            bank = pbn()
            for k in range(8):
                Hh.mm(self.pb[bank][0:M, :], w[:, k, c0:c0 + M], u[:, k, :], k == 0, k == 7, ["wab", uk], [f"pb{bank}"])
            return bank

        def nxt_ob():
            i = cnt["o"] % 4
            cnt["o"] += 1
            return obf[i], f"ob{i}"

        def fmrot(bX, M):
            xb, xk = xbf[cnt["x"] % 2], f"xb{cnt['x'] % 2}"
            cnt["x"] += 1
            Hh.copy("act", xb[0:M, :], self.pb[bX][0:M, :], [f"pb{bX}", xk], [xk])
            bR = pbn()
            Hh.mm(self.pb[bR][0:M, :], perm[0:M, 0:M], xb[0:M, :], True, True, ["perm", xk], [f"pb{bR}"])
            return bR

        def rope_chunk(u, uk, cX, cR, M, tab, tk, ci, si, dest):
            bX = fm(u, uk, cX, M)
            bR = fmrot(bX, M)
            i = cnt["t"] % 3
            cnt["t"] += 1
            Hh.tt("dve", tmpA[i][0:M, :], self.pb[bX][0:M, :], tab[0:M, ci, :], ALU.mult, [f"pb{bX}", tk, f"tA{i}"], [f"tA{i}"])
            Hh.tt("dve", tmpB[i][0:M, :], self.pb[bR][0:M, :], tab[0:M, si, :], ALU.mult, [f"pb{bR}", tk, f"tB{i}"], [f"tB{i}"])
            o, ok = nxt_ob()
            Hh.tt("pool", o[0:M, :], tmpA[i][0:M, :], tmpB[i][0:M, :], ALU.add, [f"tA{i}", f"tB{i}", ok], [ok])
            if dest is not None:
                for (dap, r0, r1) in dest:
                    Hh.dma(dap, o[r0:r1, :], [ok], [])
            return o, ok

        for t in range(NQ):
            u, uk = uT[t % 2], f"uT{t % 2}"
            tab, tk = tabs[t % 2], f"tab{t % 2}"
            ts_ = slice(t * 512, (t + 1) * 512)
            if t == 0:
                Hh.dma(tab[:], self.ropetab[:, :, ts_].rearrange("f p t -> p f t"), [], [tk])
                for sub in range(4):
                    Hh.dma(hs[sub][:], h_in[sub * 128:(sub + 1) * 128, :], [], [f"hs{sub}"])
            for sub in range(4):
                sc = scr[sub % 2]
                sc["hkey"] = f"hs{sub}"
                sc["uTkey"] = uk
                self.rmsnorm_T(hs[sub][:], sub, u, sub * 128, None, f"p{sub % 2}", pbn, sc)
            if t + 1 < NQ:
                tsn = slice((t + 1) * 512, (t + 2) * 512)
                Hh.dma(tabs[(t + 1) % 2][:], self.ropetab[:, :, tsn].rearrange("f p t -> p f t"), [], [f"tab{(t + 1) % 2}"])
                for sub in range(4):
                    r0 = (t + 1) * 512 + sub * 128
                    Hh.dma(hs[sub][:], h_in[r0:r0 + 128, :], [], [f"hs{sub}"])
            for c in range(4):
                rope_chunk(u, uk, O["aq"] + c * 128, O["aq_r"] + c * 128, 128, tab, tk, 0, 1,
                           [(d["QA"][2 * c, :, ts_], 0, 64), (d["QA"][2 * c + 1, :, ts_], 64, 128)])
                rope_chunk(u, uk, O["aqi"] + c * 128, O["aqi_r"] + c * 128, 128, tab, tk, 2, 3,
                           [(d["QI"][2 * c, :, ts_], 0, 64), (d["QI"][2 * c + 1, :, ts_], 64, 128)])
            o, ok = rope_chunk(u, uk, O["akr"], O["akr_r"], 16, tab, tk, 2, 3, None)
            Hh.copy("pool", akrT[:], o[0:16, :], [ok, "akrT"], ["akrT"])
            bX = fm(u, uk, O["aki"], 64)
            bR = fmrot(bX, 64)
            Hh.act(sq[0][0:64, :], self.pb[bX][0:64, :], AF.Square, [f"pb{bX}", "sq0"], ["sq0"])
            bM = pbn()
            Hh.mm(self.pb[bM][0:64, :], ones[0:64, 0:64], sq[0][0:64, :], True, True, ["ones", "sq0"], [f"pb{bM}"])
            Hh.act(rbc[0:64, :], self.pb[bM][0:64, :], AF.Sqrt, [f"pb{bM}", "epsc", "rbc"], ["rbc"], bias=self.epsc[0:64, 0:1], scale=1.0 / 64)
            Hh.recip(rbc2[0:64, :], rbc[0:64, :], ["rbc", "rbc2"], ["rbc2"])
            i = cnt["t"] % 3
            cnt["t"] += 1
            Hh.stt("dve", tmpA[i][0:64, :], self.pb[bX][0:64, :], gki[:, 0:1], tab[0:64, 2, :], ALU.mult, ALU.mult,
                   [f"pb{bX}", "gki", tk, f"tA{i}"], [f"tA{i}"])
            Hh.stt("dve", tmpB[i][0:64, :], self.pb[bR][0:64, :], gki[:, 1:2], tab[0:64, 3, :], ALU.mult, ALU.mult,
                   [f"pb{bR}", "gki", tk, f"tB{i}"], [f"tB{i}"])
            Hh.tt("pool", tmpA[i][0:64, :], tmpA[i][0:64, :], tmpB[i][0:64, :], ALU.add, [f"tA{i}", f"tB{i}"], [f"tA{i}"])
            o, ok = nxt_ob()
            Hh.tt("pool", o[0:64, :], tmpA[i][0:64, :], rbc2[0:64, :], ALU.mult, [f"tA{i}", "rbc2", ok], [ok])
            Hh.dma(d["KI"][:, ts_], o[0:64, :], [ok], [])
            b0 = fm(u, uk, O["ackv"], 128)
            b1 = fm(u, uk, O["ackv"] + 128, 128)
            Hh.act(sq[0][:], self.pb[b0][:], AF.Square, [f"pb{b0}", "sq0"], ["sq0"])
            Hh.act(sq[1][:], self.pb[b1][:], AF.Square, [f"pb{b1}", "sq1"], ["sq1"])
            bM = pbn()
            Hh.mm(self.pb[bM][:], ones[:], sq[0][:], True, False, ["ones", "sq0"], [f"pb{bM}"])
            Hh.mm(self.pb[bM][:], ones[:], sq[1][:], False, True, ["ones", "sq1"], [f"pb{bM}"])
            Hh.act(rbc[:], self.pb[bM][:], AF.Sqrt, [f"pb{bM}", "epsc", "rbc"], ["rbc"], bias=self.epsc[:, 0:1], scale=1.0 / 256)
            Hh.recip(rbc2[:], rbc[:], ["rbc", "rbc2"], ["rbc2"])
            Hh.stt("dve", ckvT[:, 0, :], self.pb[b0][:], gkv[:, 0:1], rbc2[:], ALU.mult, ALU.mult, [f"pb{b0}", "gkv", "rbc2", "ckvT"], ["ckvT"])
            Hh.stt("dve", ckvT[:, 1, :], self.pb[b1][:], gkv[:, 1:2], rbc2[:], ALU.mult, ALU.mult, [f"pb{b1}", "gkv", "rbc2", "ckvT"], ["ckvT"])
            for h in range(8):
                bank = pbn()
                bk = f"pb{bank}"
                Hh.mm(self.pb[bank][0:64, :], wuk[:, 0, h * 64:(h + 1) * 64], ckvT[:, 0, :], True, False, ["wuk", "ckvT"], [bk])
                Hh.mm(self.pb[bank][0:64, :], wuk[:, 1, h * 64:(h + 1) * 64], ckvT[:, 1, :], False, False, ["wuk", "ckvT"], [bk])
                Hh.mm(self.pb[bank][0:64, :], self.ident[0:16, 0:64], akrT[:], False, True, ["ident", "akrT"], [bk])
                o, ok = nxt_ob()
                Hh.copy("act", o[0:64, :], self.pb[bank][0:64, :], [bk, ok], [ok])
                Hh.dma(d["KA"][h, :, ts_], o[0:64, :], [ok], [])
            for sub in range(4):
                bank = pbn()
                bk = f"pb{bank}"
                Hh.mm(self.pb[bank][:], ckvT[:, 0, sub * 128:(sub + 1) * 128], wuv[:, 0, :], True, False, ["ckvT", "wuv"], [bk])
                Hh.mm(self.pb[bank][:], ckvT[:, 1, sub * 128:(sub + 1) * 128], wuv[:, 1, :], False, True, ["ckvT", "wuv"], [bk])
                o, ok = nxt_ob()
                Hh.copy("act", o[:], self.pb[bank][:], [bk, ok], [ok])
                r0 = t * 512 + sub * 128
                Hh.dma(d["VA"][r0:r0 + 128, :], o[:], [ok], [])
            bank = pbn()
            bk = f"pb{bank}"
            for sub in range(4):
                for k in range(8):
                    Hh.mm(self.pb[bank][:, sub * 8:(sub + 1) * 8], u[:, k, sub * 128:(sub + 1) * 128], w[:, k, O["awi"]:O["awi"] + 8],
                          k == 0, k == 7, [uk, "wab"], [bk])
            Hh.ts("dve", wis[:].rearrange("p a b -> p (a b)"), self.pb[bank][:, 0:32], float(8 ** -0.5 * 64 ** -0.5), None, ALU.mult, None,
                  [bk, "wis"], ["wis"])
            Hh.dma(d["WI"][:, t * 4:(t + 1) * 4, :], wis[:], ["wis"], [])
            for c in range(4):
                for nm, dst, scl in (("bq", d["BQ"], 0.125), ("bk", d["BK"], 1.0)):
                    bank = fm(u, uk, O[nm] + c * 128, 128)
                    o, ok = nxt_ob()
                    Hh.act(o[:], self.pb[bank][:], AF.Copy, [f"pb{bank}", ok], [ok], scale=scl)
                    Hh.dma(dst[2 * c, 0:64, ts_], o[0:64, :], [ok], [])
                    Hh.dma(dst[2 * c + 1, 0:64, ts_], o[64:128, :], [ok], [])
            for sub in range(4):
                bank = pbn()
                bk = f"pb{bank}"
                for k in range(8):
                    Hh.mm(self.pb[bank][:], u[:, k, sub * 128:(sub + 1) * 128], w[:, k, O["bv"]:O["bv"] + 512], k == 0, k == 7, [uk, "wab"], [bk])
                o, ok = nxt_ob()
                Hh.copy("act", o[:], self.pb[bank][:], [bk, ok], [ok])
                r0 = t * 512 + sub * 128
                Hh.dma(d["VB"][r0:r0 + 128, :], o[:], [ok], [])
            bank = fm(u, uk, O["bf"], 8)
            i = cnt["t"] % 3
            cnt["t"] += 1
            Hh.copy("dve", tmpA[i][0:8, :], self.pb[bank][0:8, :], [f"pb{bank}", f"tA{i}"], [f"tA{i}"])
            Hh.dma(d["BFT"][:, ts_], tmpA[i][0:8, :], [f"tA{i}"], [])
        P.flush()


K.phase_proj_ab = phase_proj_ab


def load_const_bf16(self, es, name, dram, shape):
    Hh = self.Hh
    t = self.sbuf(es, name, shape, BF16)
    n = int(np.prod(shape[1:]))
    flat_t = t[:] if len(shape) == 2 else t[:].rearrange("p a b -> p (a b)")
    flat_d = dram if len(shape) == 2 else dram.rearrange("p a b -> p (a b)")
    for c0 in range(0, n, 2048):
        cw = min(2048, n - c0)
        s = self.stg[self.stgi % 2]
        sk = f"stg{self.stgi % 2}"
        self.stgi += 1
        Hh.dma(s[0:shape[0], 0:cw], flat_d[:, c0:c0 + cw], [], [sk])
        Hh.copy("pool", flat_t[:, c0:c0 + cw], s[0:shape[0], 0:cw], [sk, name], [name])
    return t


K.load_const_bf16 = load_const_bf16


def phase_decay(self, layer):
    nc, P, Hh, S = self.nc, self.P, self.Hh, self.S
    d = self.ab[layer // 2]
    NT = self.NT
    with ExitStack() as es:
        sb = lambda name, shape, dt: self.sbuf(es, name, shape, dt)
        xt = sb("xt", [8, S], F32)
        lt = sb("lt", [8, S], F32)
        cum = sb("cum", [8, S], F32)
        one = sb("one", [8, S], F32)
        t32 = sb("t32", [8, S], F32)
        hi = sb("hi", [8, S], BF16)
        lo = sb("lo", [8, S], BF16)
        lo2 = sb("lo2", [8, S], BF16)
        oneb = sb("oneb", [8, S], BF16)
        bfg = sb("bfg", [8, 2], F32)
        onec = sb("onec", [8, 1], F32)
        ncum = sb("ncum", [128, NT, 8], F32)
        Hh.dma(xt[:], d["BFT"], [], ["xt"])
        Hh.dma(bfg[:, 0:1], d["b_forget"], [], ["bfg"])
        Hh.memset("pool", one[:], 1.0, ["one"])
        Hh.memset("pool", oneb[:], 1.0, ["oneb"])
        Hh.memset("pool", onec[:], 1.0, ["onec"])
        Hh.ts("dve", bfg[:, 1:2], bfg[:, 0:1], -1.0, None, ALU.mult, None, ["bfg"], ["bfg2"])
        Hh.act(lt[:], xt[:], AF.Exp, ["xt", "bfg2", "lt"], ["lt"], bias=bfg[:, 1:2], scale=-1.0)
        Hh.act(xt[:], lt[:], AF.Ln, ["lt", "onec", "xt"], ["xt"], bias=onec[:, 0:1], scale=1.0)
        P.op("dve", lambda e: e.tensor_tensor_scan(out=cum[:], data0=one[:], data1=xt[:], initial=0.0,
                                                   op0=ALU.mult, op1=ALU.subtract), ["one", "xt"], ["cum"])
        Hh.copy("dve", hi[:], cum[:], ["cum"], ["hi"])
        Hh.copy("dve", t32[:], hi[:], ["hi"], ["t32"])
        Hh.tt("dve", t32[:], cum[:], t32[:], ALU.subtract, ["cum", "t32"], ["t32"])
        Hh.copy("dve", lo[:], t32[:], ["t32"], ["lo"])
        Hh.copy("dve", lt[:], lo[:], ["lo", "lt"], ["lt"])
        Hh.tt("dve", t32[:], t32[:], lt[:], ALU.subtract, ["t32", "lt"], ["t32"])
        Hh.copy("dve", lo2[:], t32[:], ["t32"], ["lo2"])
        Hh.dma(d["BQ"][:, 64, :], hi[:], ["hi"], [])
        Hh.dma(d["BQ"][:, 65, :], lo[:], ["lo"], [])
        Hh.dma(d["BK"][:, 64, :], oneb[:], ["oneb"], [])
        Hh.dma(d["BK"][:, 65, :], oneb[:], ["oneb"], [])
        pbn = rr([0, 1, 2, 3])
        for g in range(0, NT, 16):
            bank = pbn()
            bk = f"pb{bank}"
            n = min(16, NT - g)
            for q in range(n):
                tsl = slice((g + q) * 128, (g + q + 1) * 128)
                for ti, (src, sk) in enumerate(((hi, "hi"), (lo, "lo"), (lo2, "lo2"))):
                    Hh.mm(self.pb[bank][:, q * 8:(q + 1) * 8], src[0:8, tsl], self.ident[0:8, 0:8], ti == 0, ti == 2, [sk, "ident"], [bk])
            Hh.ts("dve", ncum[:, g:g + n, :].rearrange("p a b -> p (a b)"), self.pb[bank][:, 0:n * 8], -1.0, None, ALU.mult, None,
                  [bk, "ncum"], ["ncum"])
        Hh.dma(d["NCUM"], ncum[:], ["ncum"], [])
        P.flush()


K.phase_decay = phase_decay


def phase_idx(self, layer):
    nc, P, Hh, S = self.nc, self.P, self.Hh, self.S
    d = self.ab[layer // 2]
    NT = self.NT
    NKEEP = min(256, S // 4)
    NIT = 20
    with ExitStack() as es:
        sb = lambda name, shape, dt: self.sbuf(es, name, shape, dt)
        ki = sb("ki", [64, S], BF16)
        wi = sb("wi", [128, NT, 8], F32)
        caus = sb("caus", [128, 128], F32)
        pow2 = sb("pow2", [128, NIT], F32)
        kap = sb("kap", [128, 1], F32)
        Hh.dma(ki[:], d["KI"], [], ["ki"])
        Hh.dma(wi[:], d["WI"], [], ["wi"])
        Hh.dma(caus[:], self.c_caus, [], ["caus"])
        Hh.dma(pow2[:], self.c_pow2[:, 0:NIT], [], ["pow2"])
        Hh.memset("dve", kap[:], float(NKEEP) - 0.5, ["kap"])
        NB = 4
        qi = [sb(f"qi{i}", [64, 8, 128], BF16) for i in range(NB)]
        I = [sb(f"I{i}", [128, S], F32) for i in range(NB)]
        mb = [sb(f"mb{i}", [128, S], BF16) for i in range(NB)]
        dg = [sb(f"dg{i}", [128, 8, 128], BF16) for i in range(NB)]
        junk = [sb(f"junkb{i}", [128, S], BF16) for i in range(2)]
        r = [sb(f"r{i}", [128, 512], BF16) for i in range(4)]
        st = [dict(hi=sb(f"hi{i}", [128, 1], F32), lo=sb(f"lo{i}", [128, 1], F32), w0=sb(f"w0{i}", [128, 1], F32),
                   steps=sb(f"steps{i}", [128, NIT], F32), nst=sb(f"nst{i}", [128, NIT], F32), mid=sb(f"mid{i}", [128, 1], F32),
                   cnt=sb(f"cnt{i}", [128, 1], F32), g=sb(f"g{i}", [128, 1], F32)) for i in range(NB)]
        pbn = rr([0, 1, 2, 3, 4, 5])
        ci = [0]
        rcnt = [0]

        def indexer(i):
            p = i % NB
            Ik, qk = f"I{p}", f"qi{p}"
            L = (i + 1) * 128
            Hh.dma(qi[p][:], d["QI"][:, :, i * 128:(i + 1) * 128].rearrange("h d t -> d h t"), [], [qk])
            dgp = dg[p]
            for h in range(8):
                Hh.ts("dve", dgp[:, h, :], self.ident[:], wi[:, i, h:h + 1], None, ALU.mult, None, ["ident", "wi", f"dg{p}"], [f"dg{p}"])
            items = []
            for c in range((L + 511) // 512):
                wd = min(512, L - c * 512)
                ib = 6 + (ci[0] % 2)
                ci[0] += 1
                for h in range(8):
                    items.append((c, h, wd, ib))
            pend = {}

            def pre(it):
                c, h, wd, ib = it
                cs = slice(c * 512, c * 512 + wd)
                bank = pbn()
                bk = f"pb{bank}"
                Hh.mm(self.pb[bank][:, 0:wd], qi[p][:, h, :], ki[:, cs], True, True, [qk, "ki"], [bk])
                n = rcnt[0]
                rcnt[0] += 1
                rb, rk = r[n % 4], f"r{n % 4}"
                Hh.act(rb[:, 0:wd], self.pb[bank][:, 0:wd], AF.Relu, [bk, rk], [rk])
                pend[it] = (rb, rk)

            def post(it):
                c, h, wd, ib = it
                cs = slice(c * 512, c * 512 + wd)
                rb, rk = pend.pop(it)
                Hh.mm(self.pb[ib][:, 0:wd], dgp[:, h, :], rb[:, 0:wd], h == 0, h == 7, [f"dg{p}", rk], [f"pb{ib}"])
                if h == 7:
                    Hh.copy("act", I[p][:, cs], self.pb[ib][:, 0:wd], [f"pb{ib}", Ik], [Ik])

            for k in range(min(2, len(items))):
                pre(items[k])
            for k, it in enumerate(items):
                if k + 2 < len(items):
                    pre(items[k + 2])
                post(it)

        def bisect_steps(i, jn):
            p = i % NB
            Ik, mk = f"I{p}", f"mb{p}"
            L = (i + 1) * 128
            s = st[p]
            sk = f"st{p}"
            jb, jk = junk[jn], f"junkb{jn}"
            steps = []
            steps.append(lambda: Hh.tt("dve", I[p][:, i * 128:L], I[p][:, i * 128:L], caus[:], ALU.add, [Ik, "caus"], [Ik]))
            if i * 128 < NKEEP:
                steps.append(lambda: Hh.memset("dve", s["lo"][:], -1e29, [sk]))
            else:
                steps.append(lambda: P.op("dve", lambda e: e.reduce_max(out=s["hi"][:], in_=I[p][:, 0:L], axis=AX.X), [Ik, sk], [sk]))
                steps.append(lambda: P.op("dve", lambda e: e.tensor_reduce(out=s["lo"][:], in_=I[p][:, 0:i * 128], axis=AX.X, op=ALU.min), [Ik, sk], [sk]))
                steps.append(lambda: Hh.tt("dve", s["w0"][:], s["hi"][:], s["lo"][:], ALU.subtract, [sk], [sk]))
                steps.append(lambda: Hh.ts("dve", s["steps"][:], pow2[:], s["w0"][:, 0:1], None, ALU.mult, None, ["pow2", sk], [sk]))
                steps.append(lambda: Hh.ts("dve", s["nst"][:], s["steps"][:], -1.0, None, ALU.mult, None, [sk], [sk]))
                steps.append(lambda: Hh.tt("dve", s["mid"][:], s["lo"][:], s["steps"][:, 0:1], ALU.add, [sk], [sk]))
                for it in range(NIT):
                    steps.append(lambda: Hh.ts("dve", jb[:, 0:L], I[p][:, 0:L], s["mid"][:, 0:1], None, ALU.is_ge, ALU.add, [Ik, sk, jk], [jk, sk],
                                               accum_out=s["cnt"][:, 0:1]))
                    if it + 1 < NIT:
                        steps.append(lambda it=it: Hh.stt("dve", s["g"][:], s["cnt"][:], kap[:, 0:1], s["steps"][:, it:it + 1], ALU.is_ge, ALU.mult,
                                                          [sk, "kap"], [sk]))
                        steps.append(lambda it=it: Hh.stt("dve", s["mid"][:], s["g"][:], s["nst"][:, it + 1:it + 2], s["mid"][:], ALU.add, ALU.add,
                                                          [sk], [sk]))
                    else:
                        steps.append(lambda it=it: Hh.stt("dve", s["g"][:], s["cnt"][:], kap[:, 0:1], s["steps"][:, it:it + 1], ALU.is_ge, ALU.mult,
                                                          [sk, "kap"], [sk]))
                        steps.append(lambda it=it: Hh.stt("dve", s["lo"][:], s["g"][:], s["nst"][:, it:it + 1], s["mid"][:], ALU.add, ALU.add,
                                                          [sk], [sk]))
            steps.append(lambda: Hh.ts("dve", mb[p][:, 0:L], I[p][:, 0:L], s["lo"][:, 0:1], None, ALU.is_lt, None, [Ik, sk, mk], [mk]))
            steps.append(lambda: Hh.dma(d["MB"][i * 128:(i + 1) * 128, 0:L], mb[p][:, 0:L], [mk], []))
            return steps

        npair = (NT + 1) // 2
        pairs = [[t for t in (2 * m, 2 * m + 1) if t < NT] for m in range(npair)]
        for t in pairs[0]:
            indexer(t)
        for m in range(npair):
            if m + 1 < npair:
                for t in pairs[m + 1]:
                    indexer(t)
            lists = [bisect_steps(t, n) for n, t in enumerate(pairs[m])]
            for k in range(max(len(l) for l in lists)):
                for l in lists:
                    if k < len(l):
                        l[k]()
        P.flush()


K.phase_idx = phase_idx


def phase_attn_ab(self, layer):
    nc, P, Hh, S = self.nc, self.P, self.Hh, self.S
    d = self.ab[layer // 2]
    NT = self.NT
    with ExitStack() as es:
        sb = lambda name, shape, dt: self.sbuf(es, name, shape, dt)
        self.stg = [sb("stg0", [128, 2048], F32), sb("stg1", [128, 2048], F32)]
        self.stgi = 0
        ncum = sb("ncum", [128, NT, 8], F32)
        Hh.dma(ncum[:], d["NCUM"], [], ["kbias"])
        cm = self.load_const_bf16(es, "cm", self.c_cm, [128, 4, 512])
        negi = self.load_const_bf16(es, "negi", self.c_negi, [128, 128])
        heads = []
        for h in range(8):
            heads.append(dict(q=d["BQ"][h], k=d["BK"][h], Kd=66, v=d["VB"][:, h * 64:(h + 1) * 64], ocol=512 + h * 64,
                              kbias=(lambda kt, h=h: ncum[:, kt, h:h + 1]),
                              kts=(lambda j: list(range(4 * j + 4))),
                              terms=(lambda j, kt, par: [(self.ident[:], cm[:, kt - 4 * j, :], ("ident", "cm"))] if kt >= 4 * j else [])))
        self.attention(es, heads, 512, d["OCAT"], S, tagp="fox")
        P.flush()
    with ExitStack() as es:
        sb = lambda name, shape, dt: self.sbuf(es, name, shape, dt)
        self.stg = [sb("stg0", [128, 2048], F32), sb("stg1", [128, 2048], F32)]
        self.stgi = 0
        negi = self.load_const_bf16(es, "negi", self.c_negi, [128, 128])
        mbs = [sb(f"mbs{i}", [128, S], BF16) for i in range(3)]

        def unit_load(hi, j, par):
            L = (j + 1) * 128
            Hh.dma(mbs[par][:, 0:L], d["MB"][j * 128:(j + 1) * 128, 0:L], [], [f"mbs{par}"])

        heads = []
        for h in range(8):
            heads.append(dict(q=d["QA"][h], k=d["KA"][h], Kd=64, v=d["VA"][:, h * 64:(h + 1) * 64], ocol=h * 64,
                              kts=(lambda j: list(range(j + 1))),
                              terms=(lambda j, kt, par: [(mbs[par][:, kt * 128:(kt + 1) * 128], negi[:], (f"mbs{par}", "negi"))])))
        self.attention(es, heads, 128, d["OCAT"], S, unit_load=unit_load, tagp="dsa")
        P.flush()


K.phase_attn_ab = phase_attn_ab


def phase_out(self, srcs, wout_d, h_in, h_out):
    nc, P, Hh, S = self.nc, self.P, self.Hh, self.S
    with ExitStack() as es:
        sb = lambda name, shape, dt: self.sbuf(es, name, shape, dt)
        self.stg = [sb("stg0", [128, 2048], F32), sb("stg1", [128, 2048], F32)]
        self.stgi = 0
        w = self.load_weight_bf16(es, "wout", wout_d, 8, D)
        ob = [[sb(f"o{i}_{s}", [128, D], BF16) for s in range(len(srcs))] for i in range(3)]
        hs = [sb(f"hs{i}", [128, D], F32) for i in range(3)]
        oT = [sb(f"oT{i}", [128, 8, 128], BF16) for i in range(3)]
        pbn = rr([0, 1, 2, 3, 4, 5, 6, 7])

        def out_loads(t_):
            p_ = t_ % 3
            rs_ = slice(t_ * 128, (t_ + 1) * 128)
            for s_, src_ in enumerate(srcs):
                Hh.dma(ob[p_][s_][:], src_[rs_, :], [], [f"o{p_}_{s_}"])
            Hh.dma(hs[p_][:], h_in[rs_, :], [], [f"hs{p_}"])

        for t in range(self.NT):
            p = t % 3
            rs = slice(t * 128, (t + 1) * 128)
            if t == 0:
                out_loads(0)
            if t + 1 < self.NT:
                out_loads(t + 1)
            for s in range(1, len(srcs)):
                Hh.tt("pool", ob[p][0][:], ob[p][0][:], ob[p][s][:], ALU.add, [f"o{p}_0", f"o{p}_{s}"], [f"o{p}_0"])
            for half in range(2):
                bank = pbn()
                bk = f"pb{bank}"
                for q in range(4):
                    k = half * 4 + q
                    Hh.mm(self.pb[bank][:, q * 128:(q + 1) * 128], ob[p][0][:, k * 128:(k + 1) * 128], self.ident[:], True, True,
                          [f"o{p}_0", "ident"], [bk])
                Hh.copy("act", oT[p][:, half * 4:half * 4 + 4, :], self.pb[bank][:].rearrange("p (q t) -> p q t", q=4), [bk, f"oT{p}"], [f"oT{p}"])
            for half in range(2):
                bank = pbn()
                bk = f"pb{bank}"
                for k in range(8):
                    Hh.mm(self.pb[bank][:], oT[p][:, k, :], w[:, k, half * 512:(half + 1) * 512], k == 0, k == 7, [f"oT{p}", "wout"], [bk])
                Hh.tt("dve", hs[p][:, half * 512:(half + 1) * 512], hs[p][:, half * 512:(half + 1) * 512], self.pb[bank][:], ALU.add,
                      [bk, f"hs{p}"], [f"hs{p}"])
            Hh.dma(h_out[rs, :], hs[p][:], [f"hs{p}"], [])
        P.flush()


K.phase_out = phase_out


def layer_ab(self, layer, h_in, h_out):
    d = self.ab[layer // 2]
    self.phase_proj_ab(layer, h_in)
    self.phase_decay(layer)
    self.phase_idx(layer)
    self.phase_attn_ab(layer)
    self.phase_out([d["OCAT"]], d["w_out"], h_in, h_out)


K.layer_ab = layer_ab


C_COLS = dict(q=(0, 1024), kc=(1024, 1152), vc=(1152, 1280), ks=(1280, 1408), vs=(1408, 1536), kw=(1536, 1664),
              vw=(1664, 1792), gl=(1792, 1840))


def c_weight_layout(w_in):
    g = lambda n: w_in[:, C_COLS[n][0]:C_COLS[n][1]]
    blocks = [("q", g("q")), ("q_r", rot_cols(g("q"), 16, 64)), ("kc", g("kc")), ("kc_r", rot_cols(g("kc"), 2, 64)),
              ("ks", g("ks")), ("ks_r", rot_cols(g("ks"), 2, 64)), ("kw", g("kw")), ("kw_r", rot_cols(g("kw"), 2, 64)),
              ("vc", g("vc")), ("vs", g("vs")), ("vw", g("vw")), ("gl", g("gl"))]
    offs, c = {}, 0
    for n, b in blocks:
        offs[n] = c
        c += b.shape[1]
    return np.ascontiguousarray(np.concatenate([b for _, b in blocks], axis=1)), offs, c


C_OFFS = c_weight_layout(np.zeros((1, 1840), np.float32))[1]
C_NC = c_weight_layout(np.zeros((1, 1840), np.float32))[2]


def decl_c(self, j):
    S, NT = self.S, self.NT
    d = {}
    d["wc"] = self.din(f"c_w_{j}", [D, C_NC])
    d["g_attn"] = self.din(f"c_g_{j}", [128, 8])
    d["b_gate"] = self.din(f"c_bg_{j}", [128, 48])
    d["pe_k"] = self.din(f"c_pek_{j}", [128, 2048])
    d["pe_v"] = self.din(f"c_pev_{j}", [128, 2048])
    d["wk1"] = self.din(f"c_wk1_{j}", [2048, 128])
    d["wk2"] = self.din(f"c_wk2_{j}", [128, 64])
    d["wv1"] = self.din(f"c_wv1_{j}", [2048, 128])
    d["wv2"] = self.din(f"c_wv2_{j}", [128, 64])
    d["w_out"] = self.din(f"c_wout_{j}", [D, D])
    for nm, shp, dt in (("QC", [16, 64, S], BF16), ("KCg", [2, S, 64], BF16), ("VCg", [2, S, 64], BF16),
                        ("KS", [2, 64, S], BF16), ("VS", [S, 128], BF16), ("KW", [2, 64, S], BF16), ("VW", [S, 128], BF16),
                        ("G", [128, NT, 48], F32), ("KCMP", [2, 64, 256], BF16), ("VCMP", [2, 256, 64], BF16),
                        ("SBT", [2, 64, S], BF16), ("EBF", [64, S], BF16), ("OC", [S, D], BF16), ("OS", [S, D], BF16), ("OW", [S, D], BF16)):
        d[nm] = self.dscr(f"{nm}_{j}", shp, dt)
    self.cl[j] = d
    if not hasattr(self, "c_cmpm"):
        self.c_cmpm = self.din("c_cmpm", [128, 5, 512])
        self.c_wm = self.din("c_wm", [128, 4, 512])
        self.c_E = self.din("c_E", [64, S])
        self.c_F = self.din("c_F", [128, 503])
        self.c_PA = self.din("c_PA", [128, 127])
        self.c_PB = self.din("c_PB", [128, 127])


K.decl_c = decl_c


def host_inputs_c(m, S, inp, l):
    j = l // 2
    m[f"c_w_{j}"] = c_weight_layout(inp["c_w_in"][j])[0]
    m[f"c_g_{j}"] = np.ascontiguousarray(inp["attn_norm"][l].reshape(8, 128).T)
    m[f"c_bg_{j}"] = np.ascontiguousarray(np.broadcast_to(inp["c_b_gate"][j][None, :], (128, 48)))
    m[f"c_pek_{j}"] = np.ascontiguousarray(np.broadcast_to(inp["c_pe_k"][j].reshape(1, 2048), (128, 2048)))
    m[f"c_pev_{j}"] = np.ascontiguousarray(np.broadcast_to(inp["c_pe_v"][j].reshape(1, 2048), (128, 2048)))
    m[f"c_wk1_{j}"] = np.ascontiguousarray(inp["c_cmp_k_w1"][j])
    m[f"c_wk2_{j}"] = np.ascontiguousarray(inp["c_cmp_k_w2"][j])
    m[f"c_wv1_{j}"] = np.ascontiguousarray(inp["c_cmp_v_w1"][j])
    m[f"c_wv2_{j}"] = np.ascontiguousarray(inp["c_cmp_v_w2"][j])
    m[f"c_wout_{j}"] = np.ascontiguousarray(inp["c_w_out"][j])
    if "c_cmpm" not in m:
        p = np.arange(128)[:, None, None]
        c = np.arange(512)[None, None, :]
        mm = np.arange(5)[None, :, None]
        m["c_cmpm"] = np.where(16 * p + 31 - 512 * mm <= c, 0.0, NEG).astype(np.float32)
        r = np.arange(4)[None, :, None]
        m["c_wm"] = np.where(128 * r + p > c, 0.0, NEG).astype(np.float32)
        m["c_E"] = (np.arange(S)[None, :] // 64 == np.arange(64)[:, None]).astype(np.float32)
        mp = np.arange(503)[None, :] - 248
        pp = np.arange(128)[:, None]
        m["c_F"] = np.where(16 * mp + 31 <= pp, 0.0, NEG).astype(np.float32)
        jp = np.arange(127)[None, :] - 63
        hi = (pp >= 64)
        PA = np.ones((128, 127), np.float32)
        PB = np.zeros((128, 127), np.float32)
        f_m1 = (jp == -1) & (~hi)
        f_0 = (jp == 0)
        f_1 = (jp == 1) & hi
        inv = ((jp == 1) & (~hi)) | (jp >= 2)
        PA[f_m1 | f_0 | f_1 | inv] = 0.0
        PB[np.broadcast_to(f_m1, PB.shape)] = 10000.0
        PB[np.broadcast_to(f_0, PB.shape)] = 10001.0
        PB[np.broadcast_to(f_1, PB.shape)] = 10002.0
        PB[np.broadcast_to(inv, PB.shape)] = -1e30
        m["c_PA"] = PA
        m["c_PB"] = PB


def phase_proj_c(self, layer, h_in):
    nc, P, Hh, S = self.nc, self.P, self.Hh, self.S
    d = self.cl[layer // 2]
    O = C_OFFS
    NQ = S // 512
    with ExitStack() as es:
        sb = lambda name, shape, dt: self.sbuf(es, name, shape, dt)
        self.stg = [sb("stg0", [128, 2048], F32), sb("stg1", [128, 2048], F32)]
        self.stgi = 0
        gain = sb("gain", [128, 8], F32)
        Hh.dma(gain[:], d["g_attn"], [], ["gain"])
        w = self.load_weight_bf16(es, "wc", d["wc"], 8, C_NC, gain=gain)
        bg = sb("bg", [128, 48], F32)
        Hh.dma(bg[:], d["b_gate"], [], ["bg"])
        hs = [sb(f"hs{i}", [128, D], F32) for i in range(4)]
        uT = [sb(f"uT{i}", [128, 8, 512], BF16) for i in range(2)]
        scr = [dict(ss=sb(f"ss{i}", [128, 1], F32), rstd=sb(f"rstd{i}", [128, 2], F32),
                    ub=sb(f"ub{i}", [128, D], BF16), junk=sb(f"junk{i}", [128, D], F32)) for i in range(2)]
        tabs = [sb(f"tab{i}", [128, 4, 512], F32) for i in range(2)]
        tmpA = [sb(f"tA{i}", [128, 512], F32) for i in range(3)]
        tmpB = [sb(f"tB{i}", [128, 512], F32) for i in range(3)]
        obf = [sb(f"ob{i}", [128, 512], BF16) for i in range(4)]
        xbf = [sb(f"xb{i}", [128, 512], BF16) for i in range(2)]
        perm = self.load_const_bf16(es, "perm", self.c_perm, [128, 128])
        gs = sb("gs", [128, 4, 48], F32)
        pbn = rr([0, 1, 2, 3, 4, 5, 6, 7])
        cnt = dict(t=0, o=0, x=0)

        def fm(u, uk, c0, M):
            bank = pbn()
            for k in range(8):
                Hh.mm(self.pb[bank][0:M, :], w[:, k, c0:c0 + M], u[:, k, :], k == 0, k == 7, ["wc", uk], [f"pb{bank}"])
            return bank

        def nxt_ob():
            i = cnt["o"] % 4
            cnt["o"] += 1
            return obf[i], f"ob{i}"

        def rope_chunk(u, uk, cX, cR, tab, tk, ci, si):
            bX = fm(u, uk, cX, 128)
            xb, xk = xbf[cnt["x"] % 2], f"xb{cnt['x'] % 2}"
            cnt["x"] += 1
            Hh.copy("act", xb[:], self.pb[bX][:], [f"pb{bX}", xk], [xk])
            bR = pbn()
            Hh.mm(self.pb[bR][:], perm[:], xb[:], True, True, ["perm", xk], [f"pb{bR}"])
            i = cnt["t"] % 3
            cnt["t"] += 1
            Hh.tt("dve", tmpA[i][:], self.pb[bX][:], tab[:, ci, :], ALU.mult, [f"pb{bX}", tk, f"tA{i}"], [f"tA{i}"])
            Hh.tt("dve", tmpB[i][:], self.pb[bR][:], tab[:, si, :], ALU.mult, [f"pb{bR}", tk, f"tB{i}"], [f"tB{i}"])
            o, ok = nxt_ob()
            Hh.tt("pool", o[:], tmpA[i][:], tmpB[i][:], ALU.add, [f"tA{i}", f"tB{i}", ok], [ok])
            return o, ok

        for t in range(NQ):
            u, uk = uT[t % 2], f"uT{t % 2}"
            tab, tk = tabs[t % 2], f"tab{t % 2}"
            ts_ = slice(t * 512, (t + 1) * 512)
            if t == 0:
                Hh.dma(tab[:], self.ropetab[:, :, ts_].rearrange("f p t -> p f t"), [], [tk])
                for sub in range(4):
                    Hh.dma(hs[sub][:], h_in[sub * 128:(sub + 1) * 128, :], [], [f"hs{sub}"])
            for sub in range(4):
                sc = scr[sub % 2]
                sc["hkey"] = f"hs{sub}"
                sc["uTkey"] = uk
                self.rmsnorm_T(hs[sub][:], sub, u, sub * 128, None, f"p{sub % 2}", pbn, sc)
            if t + 1 < NQ:
                tsn = slice((t + 1) * 512, (t + 2) * 512)
                Hh.dma(tabs[(t + 1) % 2][:], self.ropetab[:, :, tsn].rearrange("f p t -> p f t"), [], [f"tab{(t + 1) % 2}"])
                for sub in range(4):
                    r0 = (t + 1) * 512 + sub * 128
                    Hh.dma(hs[sub][:], h_in[r0:r0 + 128, :], [], [f"hs{sub}"])
            for c in range(8):
                o, ok = rope_chunk(u, uk, O["q"] + c * 128, O["q_r"] + c * 128, tab, tk, 0, 1)
                Hh.dma(d["QC"][2 * c, :, ts_], o[0:64, :], [ok], [])
                Hh.dma(d["QC"][2 * c + 1, :, ts_], o[64:128, :], [ok], [])
            for nm, dst in (("ks", d["KS"]), ("kw", d["KW"])):
                o, ok = rope_chunk(u, uk, O[nm], O[nm + "_r"], tab, tk, 2, 3)
                Hh.dma(dst[0, :, ts_], o[0:64, :], [ok], [])
                Hh.dma(dst[1, :, ts_], o[64:128, :], [ok], [])
            o, ok = rope_chunk(u, uk, O["kc"], O["kc_r"], tab, tk, 2, 3)
            bank = pbn()
            bk = f"pb{bank}"
            for sub in range(4):
                Hh.mm(self.pb[bank][:, sub * 128:(sub + 1) * 128], o[:, sub * 128:(sub + 1) * 128], self.ident[:], True, True, [ok, "ident"], [bk])
            o2, ok2 = nxt_ob()
            Hh.copy("act", o2[:], self.pb[bank][:], [bk, ok2], [ok2])
            for sub in range(4):
                r0 = t * 512 + sub * 128
                for g in range(2):
                    Hh.dma(d["KCg"][g, r0:r0 + 128, :], o2[:, sub * 128 + g * 64:sub * 128 + g * 64 + 64], [ok2], [])
            for sub in range(4):
                r0 = t * 512 + sub * 128
                bank = pbn()
                bk = f"pb{bank}"
                for k in range(8):
                    Hh.mm(self.pb[bank][:, 0:432], u[:, k, sub * 128:(sub + 1) * 128], w[:, k, O["vc"]:O["vc"] + 432], k == 0, k == 7, [uk, "wc"], [bk])
                o, ok = nxt_ob()
                Hh.copy("act", o[:, 0:384], self.pb[bank][:, 0:384], [bk, ok], [ok])
                for g in range(2):
                    Hh.dma(d["VCg"][g, r0:r0 + 128, :], o[:, g * 64:(g + 1) * 64], [ok], [])
                Hh.dma(d["VS"][r0:r0 + 128, :], o[:, 128:256], [ok], [])
                Hh.dma(d["VW"][r0:r0 + 128, :], o[:, 256:384], [ok], [])
                Hh.tt("dve", gs[:, sub, :], self.pb[bank][:, 384:432], bg[:], ALU.add, [bk, "bg", "gs"], ["gs"])
            Hh.act(gs[:].rearrange("p a b -> p (a b)"), gs[:].rearrange("p a b -> p (a b)"), AF.Sigmoid, ["gs"], ["gs"])
            Hh.dma(d["G"][:, t * 4:(t + 1) * 4, :], gs[:], ["gs"], [])
        P.flush()


K.phase_proj_c = phase_proj_c


def phase_cmp(self, layer):
    nc, P, Hh, S = self.nc, self.P, self.Hh, self.S
    d = self.cl[layer // 2]
    NCMP = S // 16 - 1
    ntl = [(0, min(128, NCMP))] + ([(128, NCMP - 128)] if NCMP > 128 else [])
    with ExitStack() as es:
        sb = lambda name, shape, dt: self.sbuf(es, name, shape, dt)
        self.stg = [sb("stg0", [128, 2048], F32), sb("stg1", [128, 2048], F32)]
        self.stgi = 0
        w1 = {"k": self.load_weight_bf16(es, "wk1", d["wk1"], 16, 128), "v": self.load_weight_bf16(es, "wv1", d["wv1"], 16, 128)}
        w2 = {"k": self.load_weight_bf16(es, "wk2", d["wk2"], 1, 64), "v": self.load_weight_bf16(es, "wv2", d["wv2"], 1, 64)}
        pe = {"k": sb("pek", [128, 2048], F32), "v": sb("pev", [128, 2048], F32)}
        Hh.dma(pe["k"][:], d["pe_k"], [], ["pek"])
        Hh.dma(pe["v"][:], d["pe_v"], [], ["pev"])
        X = [sb(f"X{i}", [128, 2048], BF16) for i in range(2)]
        Xp = [sb(f"Xp{i}", [128, 2048], BF16) for i in range(2)]
        XT = sb("XT", [128, 16, 256], BF16)
        hx = sb("hx", [128, 256], F32)
        t1 = sb("t1", [128, 256], F32)
        t2 = sb("t2", [128, 256], F32)
        gl = sb("gl", [128, 256], BF16)
        oc = sb("oc", [128, 256], BF16)
        pbn = rr([0, 1, 2, 3, 4, 5, 6, 7])
        xi = 0
        for g in range(2):
            for kv, src in (("k", d["KCg"]), ("v", d["VCg"])):
                V = src[g].rearrange("(a b) e -> a (b e)", b=16)
                for (n0, nr) in ntl:
                    p = xi % 2
                    xi += 1
                    Hh.dma(X[p][0:nr, 0:1024], V[n0:n0 + nr, :], [], [f"X{p}"])
                    Hh.dma(X[p][0:nr, 1024:2048], V[n0 + 1:n0 + 1 + nr, :], [], [f"X{p}"])
                    Hh.tt("pool", Xp[p][0:nr, :], X[p][0:nr, :], pe[kv][0:nr, :], ALU.add, [f"X{p}", "pe" + kv, f"Xp{p}"], [f"Xp{p}"])
                    for q4 in range(4):
                        bank = pbn()
                        bk = f"pb{bank}"
                        for q in range(4):
                            c = q4 * 4 + q
                            Hh.mm(self.pb[bank][:, q * 128:q * 128 + nr], Xp[p][0:nr, c * 128:(c + 1) * 128], self.ident[0:nr, 0:nr], True, True,
                                  [f"Xp{p}", "ident"], [bk])
                        Hh.copy("act" if q4 % 2 else "dve", XT[:, q4 * 4:q4 * 4 + 4, n0:n0 + nr],
                                self.pb[bank][:].rearrange("p (q t) -> p q t", q=4)[:, :, 0:nr], [bk, "XT"], ["XT"])
                bank = pbn()
                bk = f"pb{bank}"
                for c in range(16):
                    Hh.mm(self.pb[bank][:, 0:NCMP], w1[kv][:, c, :], XT[:, c, 0:NCMP], c == 0, c == 15, ["w" + kv + "1", "XT"], [bk])
                N = NCMP
                Hh.copy("dve", hx[:, 0:N], self.pb[bank][:, 0:N], [bk, "hx"], ["hx"])
                Hh.tt("dve", t1[:, 0:N], hx[:, 0:N], hx[:, 0:N], ALU.mult, ["hx", "t1"], ["t1"])
                Hh.ts("dve", t1[:, 0:N], t1[:, 0:N], 0.044715, 1.0, ALU.mult, ALU.add, ["t1"], ["t1"])
                Hh.tt("dve", t1[:, 0:N], t1[:, 0:N], hx[:, 0:N], ALU.mult, ["t1", "hx"], ["t1"])
                Hh.act(t2[:, 0:N], t1[:, 0:N], AF.Tanh, ["t1", "t2"], ["t2"], scale=0.7978845608028654)
                Hh.stt("dve", t2[:, 0:N], t2[:, 0:N], 1.0, hx[:, 0:N], ALU.add, ALU.mult, ["t2", "hx"], ["t2"])
                Hh.ts("dve", gl[:, 0:N], t2[:, 0:N], 0.5, None, ALU.mult, None, ["t2", "gl"], ["gl"])
                if kv == "k":
                    bank = pbn()
                    bk = f"pb{bank}"
                    Hh.mm(self.pb[bank][0:64, 0:N], w2["k"][:, 0, :], gl[:, 0:N], True, True, ["wk2", "gl"], [bk])
                    Hh.copy("act", oc[0:64, 0:N], self.pb[bank][0:64, 0:N], [bk, "oc"], ["oc"])
                    Hh.dma(d["KCMP"][g, :, 0:N], oc[0:64, 0:N], ["oc"], [])
                else:
                    for (n0, nr) in ntl:
                        bank = pbn()
                        bk = f"pb{bank}"
                        Hh.mm(self.pb[bank][0:nr, 0:64], gl[:, n0:n0 + nr], w2["v"][:, 0, :], True, True, ["wv2", "gl"], [bk])
                        Hh.copy("act", oc[0:nr, 0:64], self.pb[bank][0:nr, 0:64], [bk, "oc"], ["oc"])
                        Hh.dma(d["VCMP"][g, n0:n0 + nr, :], oc[0:nr, 0:64], ["oc"], [])
        P.flush()


K.phase_cmp = phase_cmp


def phase_imp(self, layer):
    nc, P, Hh, S = self.nc, self.P, self.Hh, self.S
    d = self.cl[layer // 2]
    NT = self.NT
    NCMP = S // 16 - 1
    NSEL = S // 64
    NKEEP = min(16, NSEL)
    with ExitStack() as es:
        sb = lambda name, shape, dt: self.sbuf(es, name, shape, dt)
        self.stg = [sb("stg0", [128, 2048], F32), sb("stg1", [128, 2048], F32)]
        self.stgi = 0
        F = self.load_const_bf16(es, "Fm", self.c_F, [128, 503])
        negi = self.load_const_bf16(es, "negi", self.c_negi, [128, 128])
        Eb = self.load_const_bf16(es, "Eb", self.c_E, [64, S])
        Hh.dma(d["EBF"], Eb[:], ["Eb"], [])
        PA = sb("PA", [128, 127], F32)
        PB = sb("PB", [128, 127], F32)
        Hh.dma(PA[:], self.c_PA, [], ["PA"])
        Hh.dma(PB[:], self.c_PB, [], ["PB"])
        kc = sb("kc", [64, 2, 256], BF16)
        Hh.dma(kc[:, :, 0:NCMP], d["KCMP"][:, :, 0:NCMP].rearrange("g d n -> d g n"), [], ["kc"])
        qt = [sb(f"qt{i}", [64, 16, 128], BF16) for i in range(2)]
        Psum = [sb(f"Ps{i}", [128, 264], F32) for i in range(2)]
        ex8 = [sb(f"ex8{i}", [128, 8, 256], F32) for i in range(2)]
        sm8 = [sb(f"sm8{i}", [128, 16], F32) for i in range(2)]
        imp = sb("imp", [128, 64], F32)
        imp2 = sb("imp2", [128, 64], F32)
        m8 = sb("m8", [128, 16], F32)
        thr = sb("thr", [128, 1], F32)
        selb = sb("selb", [128, 64], BF16)
        sbt = sb("sbt", [64, 128], BF16)
        pbn = rr([0, 1, 2, 3, 4, 5, 6, 7])
        ei = 0
        for i in range(NT):
            p = i % 2
            if i == 0:
                Hh.dma(qt[0][:], d["QC"][:, :, 0:128].rearrange("h d t -> d h t"), [], ["qt0"])
            if i + 1 < NT:
                Hh.dma(qt[(i + 1) % 2][:], d["QC"][:, :, (i + 1) * 128:(i + 2) * 128].rearrange("h d t -> d h t"), [], [f"qt{(i + 1) % 2}"])
            N = min(NCMP, 8 * i + 7)
            for g in range(2):
                Pk = f"Ps{g}"
                Hh.memset("pool", Psum[g][:], 0.0, [Pk])
                for hh in range(8):
                    h = g * 8 + hh
                    bank = pbn()
                    bk = f"pb{bank}"
                    Hh.mm(self.pb[bank][:, 0:N], qt[p][:, h, :], kc[:, g, 0:N], True, False, [f"qt{p}", "kc"], [bk])
                    Hh.mm(self.pb[bank][:, 0:N], self.ident[:], F[:, 248 - 8 * i:248 - 8 * i + N], False, True, ["ident", "Fm"], [bk])
                    Hh.act(ex8[g][:, hh, 0:N], self.pb[bank][:, 0:N], AF.Exp, [bk, f"ex8{g}"], [f"ex8{g}"], accum_out=sm8[g][:, hh:hh + 1])
                Hh.ts("dve", sm8[g][:, 8:16], sm8[g][:, 0:8], 1e-30, None, ALU.max, None, [f"ex8{g}"], [f"sm8{g}"])
                Hh.recip(sm8[g][:, 8:16], sm8[g][:, 8:16], [f"sm8{g}"], [f"sm8{g}"])
                for hh in range(8):
                    Hh.stt("dve", Psum[g][:, 1:1 + N], ex8[g][:, hh, 0:N], sm8[g][:, 8 + hh:9 + hh], Psum[g][:, 1:1 + N], ALU.mult, ALU.add,
                           [f"ex8{g}", f"sm8{g}", Pk], [Pk])
                Bv = Psum[g][:, 0:256].rearrange("p (j f) -> p j f", f=4)
                Hh.tt("dve", imp[:, 0:NSEL], Bv[:, 0:NSEL, 0], Bv[:, 0:NSEL, 1], ALU.add, [Pk, "imp"], ["imp"])
                Hh.tt("dve", imp[:, 0:NSEL], imp[:, 0:NSEL], Bv[:, 0:NSEL, 2], ALU.add, [Pk, "imp"], ["imp"])
                Hh.tt("dve", imp[:, 0:NSEL], imp[:, 0:NSEL], Bv[:, 0:NSEL, 3], ALU.add, [Pk, "imp"], ["imp"])
                B4 = Psum[g][:, 4:260].rearrange("p (j f) -> p j f", f=4)
                Hh.tt("dve", imp[:, 0:NSEL], imp[:, 0:NSEL], B4[:, 0:NSEL, 0], ALU.add, [Pk, "imp"], ["imp"])
                o0 = 63 - 2 * i
                Hh.tt("dve", imp[:, 0:NSEL], imp[:, 0:NSEL], PA[:, o0:o0 + NSEL], ALU.mult, ["imp", "PA"], ["imp"])
                Hh.tt("dve", imp[:, 0:NSEL], imp[:, 0:NSEL], PB[:, o0:o0 + NSEL], ALU.add, ["imp", "PB"], ["imp"])
                if i > 0:
                    Hh.memset("dve", imp[:, 0:1], 30000.0, ["imp"])
                if NSEL < 64:
                    Hh.memset("dve", imp[:, NSEL:64], -1e30, ["imp"])
                P.op("dve", lambda e_: e_.max(out=m8[:, 0:8], in_=imp[:]), ["imp", "m8"], ["m8"])
                P.op("dve", lambda e_: e_.match_replace(out=imp2[:], in_to_replace=m8[:, 0:8], in_values=imp[:], imm_value=-1e30),
                     ["imp", "m8", "imp2"], ["imp2"])
                P.op("dve", lambda e_: e_.max(out=m8[:, 8:16], in_=imp2[:]), ["imp2", "m8"], ["m8b"])
                if NKEEP >= 16:
                    P.op("dve", lambda e_: e_.tensor_reduce(out=thr[:], in_=m8[:, 8:16], axis=AX.X, op=ALU.min), ["m8b", "thr"], ["thr"])
                else:
                    P.op("dve", lambda e_: e_.tensor_reduce(out=thr[:], in_=m8[:, 0:NKEEP], axis=AX.X, op=ALU.min), ["m8", "m8b", "thr"], ["thr"])
                Hh.ts("dve", thr[:], thr[:], -1e29, None, ALU.max, None, ["thr"], ["thr"])
                Hh.ts("dve", selb[:], imp[:], thr[:, 0:1], None, ALU.is_lt, None, ["imp", "thr", "selb"], ["selb"])
                bank = pbn()
                bk = f"pb{bank}"
                Hh.mm(self.pb[bank][0:64, 0:128], selb[:], negi[:], True, True, ["selb", "negi"], [bk])
                Hh.copy("act", sbt[:], self.pb[bank][0:64, 0:128], [bk, "sbt"], ["sbt"])
                Hh.dma(d["SBT"][g, :, i * 128:(i + 1) * 128], sbt[:], ["sbt"], [])
        P.flush()


K.phase_imp = phase_imp


def phase_attn_c(self, layer):
    nc, P, Hh, S = self.nc, self.P, self.Hh, self.S
    d = self.cl[layer // 2]
    NT = self.NT
    NCMP = S // 16 - 1
    for br in self.cfg.get("c_br", [0, 1, 2]):
        with ExitStack() as es:
            sb = lambda name, shape, dt: self.sbuf(es, name, shape, dt)
            self.stg = [sb("stg0", [128, 2048], F32), sb("stg1", [128, 2048], F32)]
            self.stgi = 0
            G = sb("G", [128, NT, 48], F32)
            Hh.dma(G[:], d["G"], [], ["gate"])
            cm = self.load_const_bf16(es, "cm", self.c_cm, [128, 4, 512])
            heads = []
            if br == 0:
                cmpm = self.load_const_bf16(es, "cmpm", self.c_cmpm, [128, 5, 512])

                def kts0(j):
                    r = [0]
                    if NCMP > 128 and 16 * 128 + 31 <= 512 * j + 511:
                        r.append(1)
                    return r

                def terms0(j, kt, par):
                    m = j - 4 * kt
                    if m >= 5:
                        return []
                    kr = min(128, NCMP - kt * 128)
                    return [(self.ident[0:kr, 0:kr], cmpm[0:kr, m, :], ("ident", "cmpm"))]
                for h in range(16):
                    g = h // 8
                    heads.append(dict(q=d["QC"][h], k=d["KCMP"][g][:, 0:NCMP], Kd=64, v=d["VCMP"][g], ocol=h * 64,
                                      gate=(lambda j, c, h=h: G[:, 4 * j + c, h * 3:h * 3 + 1]), kts=kts0, terms=terms0))
                self.attention(es, heads, 512, d["OC"], NCMP, tagp="cmp")
            elif br == 1:

                def mk_terms(g):
                    def terms1(j, kt, par):
                        r = []
                        if kt >= 4 * j:
                            r.append((self.ident[:], cm[:, kt - 4 * j, :], ("ident", "cm")))
                        return r
                    return terms1
                for h in range(16):
                    g = h // 8
                    heads.append(dict(q=d["QC"][h], k=d["KS"][g], Kd=128, Kbase=64, q_extra=d["SBT"][g], k_extra=d["EBF"],
                                      v=d["VS"][:, g * 64:(g + 1) * 64], ocol=h * 64,
                                      gate=(lambda j, c, h=h: G[:, 4 * j + c, h * 3 + 1:h * 3 + 2]),
                                      kts=(lambda j: list(range(4 * j + 4))), terms=mk_terms(g)))
                self.attention(es, heads, 512, d["OS"], S, tagp="sel")
            else:
                wm = self.load_const_bf16(es, "wm", self.c_wm, [128, 4, 512])

                def terms2(j, kt, par):
                    if kt >= 4 * j:
                        return [(self.ident[:], cm[:, kt - 4 * j, :], ("ident", "cm"))]
                    return [(self.ident[:], wm[:, kt - (4 * j - 4), :], ("ident", "wm"))]
                for h in range(16):
                    g = h // 8
                    heads.append(dict(q=d["QC"][h], k=d["KW"][g], Kd=64, v=d["VW"][:, g * 64:(g + 1) * 64], ocol=h * 64,
                                      gate=(lambda j, c, h=h: G[:, 4 * j + c, h * 3 + 2:h * 3 + 3]),
                                      kts=(lambda j: list(range(max(0, 4 * j - 4), 4 * j + 4))), terms=terms2))
                self.attention(es, heads, 512, d["OW"], S, tagp="win")
            P.flush()


K.phase_attn_c = phase_attn_c


def layer_c(self, layer, h_in, h_out):
    d = self.cl[layer // 2]
    stop = self.cfg.get("c_stop", 99)
    self.phase_proj_c(layer, h_in)
    if stop <= 1:
        return
    self.phase_cmp(layer)
    if stop <= 2:
        return
    self.phase_imp(layer)
    if stop <= 3:
        return
    self.phase_attn_c(layer)
    self.phase_out([d["OC"], d["OS"], d["OW"]], d["w_out"], h_in, h_out)


K.layer_c = layer_c


SEQ = 4096
NCORES = 8


def kernel(**inputs):
    inp = {k: np.asarray(v) for k, v in inputs.items()}
    cfg = dict(layers=[0, 1, 2, 3], mixer=True)
    kb_ = K(SEQ, cfg)
    nc = kb_.build()
    in_maps = [host_inputs(SEQ, cfg, inp, b) for b in range(NCORES)]
    res = run_bass_kernel_spmd(nc, in_maps, core_ids=list(range(NCORES)))
    return np.stack([np.asarray(r["out"], dtype=np.float32) for r in res.results], 0)
```
